# Optimizing a Trainium2 kernel written in Bass

```python
import jax
import jax.numpy as jnp
from jax import lax
import numpy as np


D_MODEL = 2048
BATCH = 16
SEQ = 2048
DEPTH = 2

HEAD_DIM = 128
RET_HEADS = 8
RET_W = RET_HEADS * HEAD_DIM
RET_CHUNK = 128
ROPE_BASE = 10000.0
NSA_HEADS = 8
NSA_KV_HEADS = 2
NSA_Q_W = NSA_HEADS * HEAD_DIM
NSA_KV_W = NSA_KV_HEADS * HEAD_DIM
NSA_N_BRANCH = 3
CMP_LEN = 32
CMP_STRIDE = 16
SEL_BLOCK = 64
SEL_TOPK = 16
NSA_WINDOW = 512
NSA_Q_BLOCK = 16
BAND_BLOCK = 128
CONV_W = 1024
CONV_K = 31
DIL_PATTERNS = ((128, 1), (512, 4), (2048, 16))
DIL_GROUP_HEADS = 4
DIL_HEADS = DIL_GROUP_HEADS * len(DIL_PATTERNS)
DIL_QKV_W = DIL_HEADS * HEAD_DIM
DIL_OUT_W = DIL_GROUP_HEADS * HEAD_DIM
EVEN_SPLITS = (RET_W, RET_W, RET_W, RET_W, NSA_Q_W, NSA_KV_W, NSA_KV_W, NSA_KV_W, NSA_KV_W, NSA_KV_W, NSA_KV_W, NSA_N_BRANCH * NSA_HEADS, NSA_Q_W)
EVEN_IN = sum(EVEN_SPLITS)
EVEN_MIX_W = RET_W + NSA_Q_W
ODD_SPLITS = (CONV_W, CONV_W, CONV_W, DIL_QKV_W, DIL_QKV_W, DIL_QKV_W, DIL_OUT_W)
ODD_IN = sum(ODD_SPLITS)
ODD_MIX_W = CONV_W + DIL_OUT_W
N_EVEN = (DEPTH + 1) // 2
N_ODD = DEPTH // 2
NORM_EPS = 1e-6
NEG_INF = -1e30

kernel_name = 'hybrid_retnet_nsa_conformer_dilated'


def rms_norm(x, gain):
    xf = x.astype(jnp.float32)
    y = xf * lax.rsqrt(jnp.mean(xf * xf, axis=-1, keepdims=True) + NORM_EPS)
    return (y * gain.astype(jnp.float32)).astype(x.dtype)


def split_cols(h, sizes):
    offs = np.cumsum(np.array(sizes))[:-1].tolist()
    return jnp.split(h, offs, axis=-1)


def heads(t, n):
    B, S, _ = t.shape
    return t.reshape(B, S, n, -1).transpose(0, 2, 1, 3)


def merge(t):
    B, n, S, d = t.shape
    return t.transpose(0, 2, 1, 3).reshape(B, S, n * d)


def rotary(t, pos):
    d = t.shape[-1]
    half = d // 2
    inv = ROPE_BASE ** (-jnp.arange(half, dtype=jnp.float32) / half)
    ang = pos.astype(jnp.float32)[:, None] * inv[None, :]
    cos, sin = jnp.cos(ang), jnp.sin(ang)
    tf = t.astype(jnp.float32)
    t1, t2 = tf[..., :half], tf[..., half:]
    return jnp.concatenate([t1 * cos - t2 * sin, t2 * cos + t1 * sin], axis=-1).astype(t.dtype)


def retention_chunkwise(q, k, v):
    B, H, S, d = q.shape
    C = RET_CHUNK
    N = S // C
    log_g = jnp.log(1.0 - 2.0 ** (-5.0 - jnp.arange(H, dtype=jnp.float32)))
    idx = jnp.arange(C, dtype=jnp.float32)
    diff = idx[:, None] - idx[None, :]
    dmask = jnp.where(diff >= 0, jnp.exp(jnp.maximum(diff, 0.0)[None] * log_g[:, None, None]), 0.0)
    qc = q.astype(jnp.float32).reshape(B, H, N, C, d)
    kc = (k.astype(jnp.float32) * d ** -0.5).reshape(B, H, N, C, d)
    vc = v.astype(jnp.float32).reshape(B, H, N, C, d)
    scores = jnp.einsum('bhncd,bhnmd->bhncm', qc, kc) * dmask[:, None]
    o_inner = jnp.einsum('bhncm,bhnme->bhnce', scores, vc)
    k_dec = jnp.exp((C - 1 - idx)[None, :] * log_g[:, None])
    kv = jnp.einsum('bhnmd,bhnme->nbhde', kc * k_dec[:, None, :, None], vc)
    chunk_dec = jnp.exp(C * log_g)[:, None, None]

    def step(state, kv_i):
        return chunk_dec * state + kv_i, state

    _, state_prev = lax.scan(step, jnp.zeros((B, H, d, d), jnp.float32), kv)
    q_dec = jnp.exp((idx + 1.0)[None, :] * log_g[:, None])
    o_cross = jnp.einsum('bhncd,nbhde->bhnce', qc * q_dec[:, None, :, None], state_prev)
    return (o_inner + o_cross).reshape(B, H, S, d)


def banded_attention(q, k, v, max_dist, block):
    B, G, R, L, d = q.shape
    nb = -(-L // block)
    Lp = nb * block
    pad_end = Lp - L
    qp = jnp.pad(q, ((0, 0), (0, 0), (0, 0), (0, pad_end), (0, 0)))
    kp = jnp.pad(k, ((0, 0), (0, 0), (max_dist, pad_end), (0, 0)))
    vp = jnp.pad(v, ((0, 0), (0, 0), (max_dist, pad_end), (0, 0)))
    span = block + max_dist
    kidx = np.arange(nb)[:, None] * block + np.arange(span)[None, :]
    kb = kp[:, :, kidx]
    vb = vp[:, :, kidx]
    qb = qp.reshape(B, G, R, nb, block, d)
    s = jnp.einsum('bgrnqd,bgnkd->bgrnqk', qb, kb).astype(jnp.float32) * d ** -0.5
    qpos = np.arange(nb)[:, None] * block + np.arange(block)[None, :]
    kpos = kidx - max_dist
    rel = qpos[:, :, None] - kpos[:, None, :]
    mask = (rel >= 0) & (rel <= max_dist) & (kpos[:, None, :] >= 0)
    s = jnp.where(mask, s, NEG_INF)
    lse = jax.nn.logsumexp(s, axis=-1)
    p = jnp.exp(s - lse[..., None])
    o = jnp.einsum('bgrnqk,bgnkd->bgrnqd', p, vb)
    o = o.reshape(B, G, R, Lp, d)[:, :, :, :L]
    lse = lse.reshape(B, G, R, Lp)[:, :, :, :L]
    return o, lse


def compress(t, pos_emb, w1, w2):
    B, G, S, d = t.shape
    n_cmp = (S - CMP_LEN) // CMP_STRIDE + 1
    idx = np.arange(n_cmp)[:, None] * CMP_STRIDE + np.arange(CMP_LEN)[None, :]
    blk = (t[:, :, idx] + pos_emb).reshape(B, G, n_cmp, CMP_LEN * d)
    return jax.nn.gelu(blk @ w1) @ w2


def compressed_attention(q, kcmp, vcmp):
    B, G, R, S, d = q.shape
    n_cmp = kcmp.shape[2]
    s = jnp.einsum('bgrsd,bgnd->bgrsn', q, kcmp).astype(jnp.float32) * d ** -0.5
    ends = np.arange(n_cmp) * CMP_STRIDE + CMP_LEN - 1
    valid = ends[None, :] <= np.arange(S)[:, None]
    any_valid = valid.any(axis=-1).astype(np.float32)[:, None]
    s = jnp.where(valid, s, NEG_INF)
    p = jax.nn.softmax(s, axis=-1) * any_valid
    o = jnp.einsum('bgrsn,bgnd->bgrsd', p, vcmp)
    return o, p


def select_blocks(p_cmp, S):
    n_cmp = p_cmp.shape[-1]
    n_sel = S // SEL_BLOCK
    cs = np.arange(n_cmp) * CMP_STRIDE
    ss = np.arange(n_sel) * SEL_BLOCK
    overlap = ((cs[:, None] < ss[None, :] + SEL_BLOCK) & (cs[:, None] + CMP_LEN > ss[None, :])).astype(np.float32)
    imp = jnp.einsum('bgrsn,nj->bgsj', p_cmp, jnp.asarray(overlap))
    cur = np.arange(S) // SEL_BLOCK
    j = np.arange(n_sel)
    forced = (j[None, :] == 0) | (j[None, :] == cur[:, None]) | (j[None, :] == cur[:, None] - 1)
    future = j[None, :] > cur[:, None]
    imp = jnp.where(forced, -NEG_INF, jnp.where(future, NEG_INF, imp))
    _, idx = lax.top_k(imp, min(SEL_TOPK, n_sel))
    return idx


def selected_attention(q, k, v, sel_idx):
    B, G, R, S, d = q.shape
    K = sel_idx.shape[-1]
    n_sel = S // SEL_BLOCK
    kb = k.reshape(B, G, n_sel, SEL_BLOCK, d)
    vb = v.reshape(B, G, n_sel, SEL_BLOCK, d)
    nq = S // NSA_Q_BLOCK
    q_c = q.reshape(B, G, R, nq, NSA_Q_BLOCK, d).transpose(3, 0, 1, 2, 4, 5)
    i_c = sel_idx.reshape(B, G, nq, NSA_Q_BLOCK, K).transpose(2, 0, 1, 3, 4)
    t_c = jnp.arange(S).reshape(nq, NSA_Q_BLOCK)
    bi = jnp.arange(B)[:, None, None, None]
    gi = jnp.arange(G)[None, :, None, None]
    offs = jnp.arange(SEL_BLOCK)

    def one(args):
        qb, ib, tb = args
        kg = kb[bi, gi, ib]
        vg = vb[bi, gi, ib]
        s = jnp.einsum('bgrqd,bgqkld->bgrqkl', qb, kg).astype(jnp.float32) * d ** -0.5
        kpos = ib[..., None] * SEL_BLOCK + offs
        ok = kpos <= tb[:, None, None]
        s = jnp.where(ok[:, :, None], s, NEG_INF)
        shp = s.shape
        p = jax.nn.softmax(s.reshape(shp[0], shp[1], shp[2], shp[3], -1), axis=-1).reshape(shp)
        return jnp.einsum('bgrqkl,bgqkld->bgrqd', p, vg)

    o = lax.map(one, (q_c, i_c, t_c))
    return o.transpose(1, 2, 3, 0, 4, 5).reshape(B, G, R, S, d)


def nsa_attention(q, kc, vc, ks, vs, kw, vw, gate_logits, q_g, k_g, pos_k, pos_v, k_w1, k_w2, v_w1, v_w2):
    B, S, _ = q.shape
    G, R = NSA_KV_HEADS, NSA_HEADS // NSA_KV_HEADS
    qh = rms_norm(heads(q, NSA_HEADS), q_g).reshape(B, G, R, S, HEAD_DIM)
    kcmp = rms_norm(compress(heads(kc, G), pos_k, k_w1, k_w2), k_g[0])
    vcmp = compress(heads(vc, G), pos_v, v_w1, v_w2)
    o_cmp, p_cmp = compressed_attention(qh, kcmp, vcmp)
    sel_idx = select_blocks(p_cmp, S)
    o_sel = selected_attention(qh, rms_norm(heads(ks, G), k_g[1]), heads(vs, G), sel_idx)
    o_win, _ = banded_attention(qh, rms_norm(heads(kw, G), k_g[2]), heads(vw, G), NSA_WINDOW - 1, BAND_BLOCK)
    gates = jax.nn.sigmoid(gate_logits.astype(jnp.float32)).reshape(B, S, NSA_N_BRANCH, G, R).transpose(2, 0, 3, 4, 1)[..., None]
    o = gates[0] * o_cmp + gates[1] * o_sel + gates[2] * o_win
    return merge(o.reshape(B, NSA_HEADS, S, HEAD_DIM)).astype(q.dtype)


def conformer_conv(u, g, dw_w, dw_b, ln_g, ln_b):
    a = u * jax.nn.sigmoid(g)
    C = a.shape[-1]
    a = lax.conv_general_dilated(a, dw_w[:, None, :].astype(a.dtype), (1,), [(CONV_K - 1, 0)], dimension_numbers=('NWC', 'WIO', 'NWC'), feature_group_count=C) + dw_b
    af = a.astype(jnp.float32)
    mu = jnp.mean(af, axis=-1, keepdims=True)
    var = jnp.mean(jnp.square(af - mu), axis=-1, keepdims=True)
    a = ((af - mu) * lax.rsqrt(var + NORM_EPS) * ln_g + ln_b).astype(u.dtype)
    return jax.nn.silu(a)


def strided_window_attention(q, k, v, window, dil):
    B, H, S, d = q.shape
    L = S // dil

    def to_sub(t):
        return t.reshape(B, H, L, dil, d).transpose(0, 1, 3, 2, 4).reshape(B, H * dil, L, d)

    o, lse = banded_attention(to_sub(q)[:, :, None], to_sub(k), to_sub(v), window // dil, BAND_BLOCK)
    o = o[:, :, 0].reshape(B, H, dil, L, d).transpose(0, 1, 3, 2, 4).reshape(B, H, S, d)
    lse = lse[:, :, 0].reshape(B, H, dil, L).transpose(0, 1, 3, 2).reshape(B, H, S)
    return o, lse


def dilated_attention(q, k, v, q_g, k_g):
    qh = rms_norm(heads(q, DIL_HEADS), q_g)
    kh = rms_norm(heads(k, DIL_HEADS), k_g)
    vh = heads(v, DIL_HEADS)
    outs, lses = [], []
    for gi, (window, dil) in enumerate(DIL_PATTERNS):
        sl = slice(gi * DIL_GROUP_HEADS, (gi + 1) * DIL_GROUP_HEADS)
        o, lse = strided_window_attention(qh[:, sl], kh[:, sl], vh[:, sl], window, dil)
        outs.append(o)
        lses.append(lse)
    w = jax.nn.softmax(jnp.stack(lses), axis=0)
    o = jnp.sum(w[..., None] * jnp.stack(outs), axis=0)
    return merge(o).astype(q.dtype)


def even_layer(x, norm_g, w_in, w_out, ret_g, nsa_qn, nsa_kn, pos_k, pos_v, k_w1, k_w2, v_w1, v_w2):
    B, S, _ = x.shape
    h = rms_norm(x, norm_g) @ w_in
    rq, rk, rv, rz, nq, kc, vc, ks, vs, kw, vw, ng, nz = split_cols(h, EVEN_SPLITS)
    pos = jnp.arange(S)
    o = retention_chunkwise(rotary(heads(rq, RET_HEADS), pos), rotary(heads(rk, RET_HEADS), pos), heads(rv, RET_HEADS))
    o = rms_norm(o, ret_g[:, None, :]).astype(x.dtype)
    a_out = merge(o) * jax.nn.silu(rz)
    b_out = nsa_attention(nq, kc, vc, ks, vs, kw, vw, ng, nsa_qn, nsa_kn, pos_k, pos_v, k_w1, k_w2, v_w1, v_w2) * jax.nn.silu(nz)
    return x + jnp.concatenate([a_out, b_out], axis=-1) @ w_out


def odd_layer(x, norm_g, w_in, w_out, dw_w, dw_b, cn_g, cn_b, dil_qn, dil_kn):
    h = rms_norm(x, norm_g) @ w_in
    cu, cg, cz, dq, dk, dv, dz = split_cols(h, ODD_SPLITS)
    c_out = conformer_conv(cu, cg, dw_w, dw_b, cn_g, cn_b) * jax.nn.silu(cz)
    d_out = dilated_attention(dq, dk, dv, dil_qn, dil_kn) * jax.nn.silu(dz)
    return x + jnp.concatenate([c_out, d_out], axis=-1) @ w_out


def setup_inputs(seed: int = 0) -> dict:
    key = jax.random.key(seed)
    ks = jax.random.split(key, 24)

    def nrm(k, shape, scale):
        return jax.random.normal(k, shape, jnp.float32) * scale

    def gain(k, shape):
        return 1.0 + 0.02 * jax.random.normal(k, shape, jnp.float32)

    flat = CMP_LEN * HEAD_DIM
    return {
        'x': nrm(ks[0], (BATCH, SEQ, D_MODEL), 1.0),
        'ev_norm': gain(ks[1], (N_EVEN, D_MODEL)),
        'ev_w_in': nrm(ks[2], (N_EVEN, D_MODEL, EVEN_IN), D_MODEL ** -0.5),
        'ev_w_out': nrm(ks[3], (N_EVEN, EVEN_MIX_W, D_MODEL), EVEN_MIX_W ** -0.5),
        'ev_ret_norm': gain(ks[4], (N_EVEN, RET_HEADS, HEAD_DIM)),
        'ev_nsa_q_norm': gain(ks[5], (N_EVEN, HEAD_DIM)),
        'ev_nsa_k_norm': gain(ks[6], (N_EVEN, NSA_N_BRANCH, HEAD_DIM)),
        'ev_cmp_pos_k': nrm(ks[7], (N_EVEN, CMP_LEN, HEAD_DIM), 0.02),
        'ev_cmp_pos_v': nrm(ks[8], (N_EVEN, CMP_LEN, HEAD_DIM), 0.02),
        'ev_cmp_k_w1': nrm(ks[9], (N_EVEN, flat, HEAD_DIM), flat ** -0.5),
        'ev_cmp_k_w2': nrm(ks[10], (N_EVEN, HEAD_DIM, HEAD_DIM), HEAD_DIM ** -0.5),
        'ev_cmp_v_w1': nrm(ks[11], (N_EVEN, flat, HEAD_DIM), flat ** -0.5),
        'ev_cmp_v_w2': nrm(ks[12], (N_EVEN, HEAD_DIM, HEAD_DIM), HEAD_DIM ** -0.5),
        'od_norm': gain(ks[13], (N_ODD, D_MODEL)),
        'od_w_in': nrm(ks[14], (N_ODD, D_MODEL, ODD_IN), D_MODEL ** -0.5),
        'od_w_out': nrm(ks[15], (N_ODD, ODD_MIX_W, D_MODEL), ODD_MIX_W ** -0.5),
        'od_dw_w': nrm(ks[16], (N_ODD, CONV_K, CONV_W), CONV_K ** -0.5),
        'od_dw_b': nrm(ks[17], (N_ODD, CONV_W), 0.01),
        'od_conv_norm_g': gain(ks[18], (N_ODD, CONV_W)),
        'od_conv_norm_b': nrm(ks[19], (N_ODD, CONV_W), 0.01),
        'od_dil_q_norm': gain(ks[20], (N_ODD, HEAD_DIM)),
        'od_dil_k_norm': gain(ks[21], (N_ODD, HEAD_DIM)),
    }


def reference(x, ev_norm, ev_w_in, ev_w_out, ev_ret_norm, ev_nsa_q_norm, ev_nsa_k_norm, ev_cmp_pos_k, ev_cmp_pos_v, ev_cmp_k_w1, ev_cmp_k_w2, ev_cmp_v_w1, ev_cmp_v_w2, od_norm, od_w_in, od_w_out, od_dw_w, od_dw_b, od_conv_norm_g, od_conv_norm_b, od_dil_q_norm, od_dil_k_norm):
    for layer in range(DEPTH):
        i = layer // 2
        if layer % 2 == 0:
            x = even_layer(x, ev_norm[i], ev_w_in[i], ev_w_out[i], ev_ret_norm[i], ev_nsa_q_norm[i], ev_nsa_k_norm[i], ev_cmp_pos_k[i], ev_cmp_pos_v[i], ev_cmp_k_w1[i], ev_cmp_k_w2[i], ev_cmp_v_w1[i], ev_cmp_v_w2[i])
        else:
            x = odd_layer(x, od_norm[i], od_w_in[i], od_w_out[i], od_dw_w[i], od_dw_b[i], od_conv_norm_g[i], od_conv_norm_b[i], od_dil_q_norm[i], od_dil_k_norm[i])
    return x
```

```python
import math
import numpy as np
import ml_dtypes
import concourse.bass as bass
import concourse.mybir as mybir
from concourse.bass_utils import run_bass_kernel_spmd
from contextlib import ExitStack

F32 = mybir.dt.float32
BF16 = mybir.dt.bfloat16
AF = mybir.ActivationFunctionType
ALU = mybir.AluOpType

S = 2048
D = 2048
EPS = 1e-6
SCALE = 128 ** -0.5
NEGB = 30000.0


class _Op:
    __slots__ = ("eng", "fn", "deps", "dma", "sig", "val", "sem", "slot")


class Prog:
    ENGS = ("pe", "act", "dve", "pool", "sp")
    NSLOT = 8

    def __init__(self, nc):
        self.nc = nc
        self.ops = []
        self.last_w = {}
        self.readers = {}
        self.out_dmas = []
        self.last_eng = {}
        self.last_slot = {}
        self.dcnt = {e: 0 for e in self.ENGS}
        self.pending = {}

    def fence(self):
        snap = list(self.last_eng.values()) + list(self.last_slot.values())
        for e in self.ENGS:
            self.pending[e] = list(snap)
        self.last_w = {}
        self.readers = {}

    def add(self, eng, fn, reads=(), writes=(), dma=False, is_out=False):
        op = _Op()
        idx = len(self.ops)
        op.eng, op.fn, op.dma = eng, fn, dma
        op.sig = dma
        op.val = 0
        op.sem = None
        op.slot = None
        ex = [t for t in reads if t[:2] in ("PJ", "ST", "OA", "LA")]
        if ex:
            reads = [t for t in reads if t not in ex]
            writes = list(writes) + ex
        deps = set()
        for t in reads:
            w = self.last_w.get(t)
            if w is not None:
                deps.add(w)
        for t in writes:
            w = self.last_w.get(t)
            if w is not None:
                deps.add(w)
            for r in self.readers.get(t, ()):
                deps.add(r)
        pf = self.pending.pop(eng, None)
        if pf:
            deps.update(pf)
        for t in reads:
            self.readers.setdefault(t, []).append(idx)
        for t in writes:
            self.last_w[t] = idx
            self.readers[t] = []
        if dma:
            k = self.dcnt[eng]
            self.dcnt[eng] += 1
            s = k % self.NSLOT
            op.slot = (eng, s, 16 * (k // self.NSLOT + 1))
            prev = self.last_slot.get((eng, s))
            if prev is not None:
                deps.add(prev)
            self.last_slot[(eng, s)] = idx
        keep = []
        for d in deps:
            o = self.ops[d]
            if eng == "pe" and o.eng == "pe" and not o.dma and not dma:
                continue
            keep.append(d)
            o.sig = True
        op.deps = keep
        self.ops.append(op)
        if not dma:
            self.last_eng[eng] = idx
        if is_out:
            self.out_dmas.append(idx)
        return idx

    def pe(self, fn, reads=(), writes=()):
        return self.add("pe", fn, reads, writes)

    def act(self, fn, reads=(), writes=()):
        return self.add("act", fn, reads, writes)

    def dve(self, fn, reads=(), writes=()):
        return self.add("dve", fn, reads, writes)

    def pool(self, fn, reads=(), writes=()):
        return self.add("pool", fn, reads, writes)

    def dma(self, eng, fn, reads=(), writes=(), is_out=False):
        return self.add(eng, fn, reads, writes, dma=True, is_out=is_out)

    def emit(self):
        nc = self.nc
        with ExitStack() as es:
            esem = {e: es.enter_context(nc.semaphore("s_" + e)) for e in self.ENGS if e != "sp"}
            dsem = {e: [es.enter_context(nc.semaphore("d_%s%d" % (e, i))) for i in range(self.NSLOT)]
                    for e in ("sp", "pool", "act")}
            cnt = {e: 0 for e in self.ENGS}
            for op in self.ops:
                if op.dma:
                    e, s, v = op.slot
                    op.sem = dsem[e][s]
                    op.val = v
                elif op.sig:
                    cnt[op.eng] += 1
                    op.sem = esem[op.eng]
                    op.val = cnt[op.eng]
            per_eng = {e: [] for e in self.ENGS}
            for i, op in enumerate(self.ops):
                per_eng[op.eng].append(i)
            ops = self.ops
            out_dmas = self.out_dmas
            block = es.enter_context(nc.Block())

            def run(eng_name, h):
                waited = {}
                for i in per_eng[eng_name]:
                    op = ops[i]
                    need = {}
                    for d in op.deps:
                        o = ops[d]
                        key = id(o.sem)
                        if waited.get(key, 0) >= o.val:
                            continue
                        if key not in need or need[key][1] < o.val:
                            need[key] = (o.sem, o.val)
                    for key, (sem, val) in need.items():
                        h.wait_ge(sem, val)
                        waited[key] = val
                    ins = op.fn(h)
                    if op.sig:
                        ins.then_inc(op.sem, 16 if op.dma else 1)
                if eng_name == "sp":
                    fin = {}
                    for i in out_dmas:
                        o = ops[i]
                        key = id(o.sem)
                        if key not in fin or fin[key][1] < o.val:
                            fin[key] = (o.sem, o.val)
                    for key, (sem, val) in fin.items():
                        h.wait_ge(sem, val)

            @block.tensor
            def _(e):
                run("pe", e)

            @block.scalar
            def _(e):
                run("act", e)

            @block.vector
            def _(e):
                run("dve", e)

            @block.gpsimd
            def _(e):
                run("pool", e)

            @block.sync
            def _(e):
                run("sp", e)


class Rot:
    def __init__(self, items):
        self.items = items
        self.i = 0

    def next(self):
        it = self.items[self.i % len(self.items)]
        self.i += 1
        return it


def l0_cols():
    cols = []
    for h in range(8):
        cols += [128 * h, 1024 + 128 * h, 2048 + 128 * h, 3072 + 128 * h]
    for g in range(2):
        cols += [5120 + 128 * g, 5376 + 128 * g, 5632 + 128 * g, 5888 + 128 * g,
                 6144 + 128 * g, 6400 + 128 * g]
        if g == 0:
            cols += [-1]
        cols += [4096 + 128 * (4 * g + r) for r in range(4)]
        cols += [6680 + 128 * (4 * g + r) for r in range(4)]
    return cols


def l1_cols():
    cols = []
    for j in range(8):
        cols += [128 * j, 1024 + 128 * j]
    for j in range(8):
        cols += [2048 + 128 * j]
    for hh in range(4):
        for gi in range(3):
            hd = 4 * gi + hh
            cols += [3072 + 128 * hd, 3072 + 1536 + 128 * hd, 3072 + 3072 + 128 * hd]
        cols += [3072 + 4608 + 128 * hh]
    return cols


def tile_w(w, cols):
    K = w.shape[0]
    out = np.zeros((len(cols), 128, K // 128, 128), np.float32)
    for i, c in enumerate(cols):
        if c < 0:
            blk = np.zeros((K, 128), np.float32)
            blk[:, :24] = w[:, 6656:6680]
        else:
            blk = w[:, c:c + 128]
        out[i] = blk.reshape(K // 128, 128, 128).transpose(1, 0, 2)
    return out


def host_consts():
    c = {}
    bf = ml_dtypes.bfloat16
    c["idf"] = np.eye(128, dtype=np.float32)
    c["onesb"] = np.ones((128, 128), bf)
    rp = np.zeros((128, 128), np.float32)
    for m in range(128):
        rp[(m + 64) % 128, m] = 1.0
    c["rperm"] = rp.astype(bf)
    half = 64
    inv = (10000.0 ** (-np.arange(half, dtype=np.float32) / half)).astype(np.float32)
    ang = (np.arange(S, dtype=np.float32)[None, :] * inv[:, None]).astype(np.float32)
    cos, sin = np.cos(ang), np.sin(ang)
    c["rcc"] = np.concatenate([cos, cos], 0).astype(np.float32)
    c["rss"] = np.concatenate([-sin, sin], 0).astype(np.float32)
    i = np.arange(128, dtype=np.float64)[:, None]
    j = np.arange(512, dtype=np.float64)[None, :]
    rt = np.zeros((8, 128, 5, 512), np.float32)
    for h in range(8):
        lg = math.log(1.0 - 2.0 ** (-5.0 - h))
        rt[h, :, 0, :] = np.exp((j - i) * lg)
        for r in range(4):
            e = j - i - 128 * r
            rt[h, :, 1 + r, :] = np.where(e >= 0, np.exp(np.maximum(e, 0) * lg), 0.0)
    c["rtab"] = rt
    cm = np.zeros((128, 4, 512), np.float32)
    wm = np.zeros((128, 4, 512), np.float32)
    for r in range(4):
        cm[:, r, :] = (j - i - 128 * r >= 0)
        wm[:, r, :] = (j - i - 128 * (r - 4) <= 511)
    c["cm"] = cm.astype(bf)
    c["wm"] = wm.astype(bf)
    n = np.arange(S)[None, :]
    cc = np.arange(128)[:, None]
    valid = ((16 * cc + 31 <= n) & (cc < 127)).astype(np.float32)
    c["valid"] = valid.astype(bf)
    cs = np.arange(127) * 16
    ss = np.arange(32) * 64
    ovl = np.zeros((128, 32), np.float32)
    ovl[:127] = ((cs[:, None] < ss[None, :] + 64) & (cs[:, None] + 32 > ss[None, :]))
    c["ovl"] = ovl.astype(bf)
    t = np.arange(S)
    cur = t // 64
    jj = np.arange(32)
    forced = (jj[None, :] == 0) | (jj[None, :] == cur[:, None]) | (jj[None, :] == cur[:, None] - 1)
    future = jj[None, :] > cur[:, None]
    ft = np.where(forced, 1e30, np.where(future, -1e30, 0.0)).astype(np.float32)
    c["ftab"] = np.ascontiguousarray(ft.reshape(16, 128, 32).transpose(1, 0, 2))
    ex = np.zeros((32, 16, 128), np.float32)
    for kt in range(16):
        for m in range(128):
            ex[(128 * kt + m) // 64, kt, m] = NEGB
    c["expd"] = ex.astype(bf)
    sel = np.zeros((32, 24, 128), np.float32)
    for k in range(24):
        sel[k, k, :] = 1.0
    c["sel"] = sel.astype(bf)
    m_ = np.arange(128)[:, None]
    n_ = np.arange(128)[None, :]
    prev = (m_ >= n_).astype(np.float32)
    diag = (m_ <= n_).astype(np.float32)
    dm = np.zeros((128, 2, 512), np.float32)
    dm[:, 0, :] = np.concatenate([prev, diag, prev, diag], 1)
    dm[:, 1, :] = np.concatenate([np.zeros_like(prev), diag, prev, diag], 1)
    c["dm"] = dm.astype(bf)
    return c


CONST_SPECS = {
    "idf": ([128, 128], F32), "onesb": ([128, 128], BF16), "rperm": ([128, 128], BF16),
    "rcc": ([128, S], F32), "rss": ([128, S], F32), "rtab": ([8, 128, 5, 512], F32),
    "cm": ([128, 4, 512], BF16), "wm": ([128, 4, 512], BF16), "valid": ([128, S], BF16),
    "ovl": ([128, 32], BF16), "ftab": ([128, 16, 32], F32), "expd": ([32, 16, 128], BF16),
    "sel": ([32, 24, 128], BF16), "dm": ([128, 2, 512], BF16),
}


def host_params(inp):
    p = {}
    f = np.float32
    p["wl0"] = tile_w(np.asarray(inp["ev_w_in"][0], f), l0_cols())
    p["wl1"] = tile_w(np.asarray(inp["od_w_in"][0], f), l1_cols())

    def wo_l(w):
        K = w.shape[0]
        return np.ascontiguousarray(w.reshape(K // 128, 128, 4, 512).transpose(2, 1, 0, 3))

    p["wo0"] = wo_l(np.asarray(inp["ev_w_out"][0], f))
    p["wo1"] = wo_l(np.asarray(inp["od_w_out"][0], f))
    p["g0"] = np.ascontiguousarray(np.asarray(inp["ev_norm"][0], f).reshape(16, 128).T)
    p["g1"] = np.ascontiguousarray(np.asarray(inp["od_norm"][0], f).reshape(16, 128).T)
    p["retg"] = np.ascontiguousarray(np.asarray(inp["ev_ret_norm"][0], f).T)
    p["nqg"] = np.ascontiguousarray(np.asarray(inp["ev_nsa_q_norm"][0], f).reshape(128, 1))
    p["nkg"] = np.ascontiguousarray(np.asarray(inp["ev_nsa_k_norm"][0], f).T)
    p["posk"] = np.ascontiguousarray(np.asarray(inp["ev_cmp_pos_k"][0], f).T)
    p["posv"] = np.ascontiguousarray(np.asarray(inp["ev_cmp_pos_v"][0], f).T)
    p["w1k"] = np.ascontiguousarray(np.asarray(inp["ev_cmp_k_w1"][0], f).reshape(32, 128, 128).transpose(1, 0, 2))
    p["w1v"] = np.ascontiguousarray(np.asarray(inp["ev_cmp_v_w1"][0], f).reshape(32, 128, 128).transpose(1, 0, 2))
    p["w2k"] = np.ascontiguousarray(np.asarray(inp["ev_cmp_k_w2"][0], f))
    p["w2v"] = np.ascontiguousarray(np.asarray(inp["ev_cmp_v_w2"][0], f))
    p["dww"] = np.ascontiguousarray(np.asarray(inp["od_dw_w"][0], f).reshape(31, 8, 128).transpose(2, 1, 0))
    p["dwb"] = np.ascontiguousarray(np.asarray(inp["od_dw_b"][0], f).reshape(8, 128).T)
    p["cng"] = np.ascontiguousarray(np.asarray(inp["od_conv_norm_g"][0], f).reshape(8, 128).T)
    p["cnb"] = np.ascontiguousarray(np.asarray(inp["od_conv_norm_b"][0], f).reshape(8, 128).T)
    p["dqg"] = np.ascontiguousarray(np.asarray(inp["od_dil_q_norm"][0], f).reshape(128, 1))
    p["dkg"] = np.ascontiguousarray(np.asarray(inp["od_dil_k_norm"][0], f).reshape(128, 1))
    return p


PARAM_SPECS = {
    "wl0": [61, 128, 16, 128], "wl1": [64, 128, 16, 128], "wo0": [4, 128, 16, 512], "wo1": [4, 128, 12, 512],
    "g0": [128, 16], "g1": [128, 16], "retg": [128, 8], "nqg": [128, 1], "nkg": [128, 3],
    "posk": [128, 32], "posv": [128, 32], "w1k": [128, 32, 128], "w1v": [128, 32, 128],
    "w2k": [128, 128], "w2v": [128, 128], "dww": [128, 8, 31], "dwb": [128, 8], "cng": [128, 8],
    "cnb": [128, 8], "dqg": [128, 1], "dkg": [128, 1],
}


import os
ARW = int(os.environ.get("ARW", "88064"))
NW = 4
XNT_END = 16 * S


class Arena:
    def __init__(self, ap):
        self.ap = ap
        self.off = 0

    def reset(self, off=0):
        if os.environ.get("ARDBG") and self.off:
            print("arena phase end off", self.off)
        self.off = off

    def take(self, n, dt):
        nb = n * 2 if dt == F32 else n
        nb += nb % 2
        assert self.off + nb <= ARW, ("arena overflow", self.off, nb)
        a = self.ap[:, self.off:self.off + nb]
        self.off += nb
        if dt == F32:
            a = a.bitcast(F32)
        return a[:, 0:n]


def build(nseq=2, layers=(0, 1), dbg=False, phases=None):
    nc = bass.Bass("TRN2", target_bir_lowering=False)
    NT = nseq * S
    dr = {}
    x_d = nc.dram_tensor("x", [NT, D], F32, kind="ExternalInput").ap()
    out_d = nc.dram_tensor("out", [NT, D], F32, kind="ExternalOutput").ap()
    for k, (shp, dt) in CONST_SPECS.items():
        dr[k] = nc.dram_tensor("c_" + k, shp, dt, kind="ExternalInput").ap()
    for k, shp in PARAM_SPECS.items():
        dr[k] = nc.dram_tensor("p_" + k, shp, F32, kind="ExternalInput").ap()
    x1_d = nc.dram_tensor("x1s", [NT, D], F32, kind="Internal").ap()
    mix_d = nc.dram_tensor("mixs", [nseq * 2, 16, 128, S], BF16,
                           kind="ExternalOutput" if dbg else "Internal").ap()

    es = ExitStack()
    with es:
        def sb(name, shape, dt):
            return es.enter_context(nc.sbuf_tensor(name, shape, dt))

        ARt = sb("arena", [128, ARW], BF16)
        WBt = sb("wbr", [128, NW * 2048], BF16)
        IDF = sb("idf", [128, 128], F32)
        IDB = sb("idb", [128, 128], BF16)
        ONESB = sb("onesb", [128, 128], BF16)
        RPERM = sb("rperm", [128, 128], BF16)
        PRM = sb("prm", [128, 128], F32)
        psb = [es.enter_context(nc.psum_tensor("psb%d" % i, [128, 512], F32)) for i in range(8)]
        PJ = Rot([(psb[0], "PJ0"), (psb[1], "PJ1")])
        ST = Rot([(psb[2], "ST0"), (psb[3], "ST1")])
        OA = Rot([(psb[4], "OA0"), (psb[5], "OA1")])
        LA = Rot([(psb[6], "LA0"), (psb[7], "LA1")])
        ar = Arena(ARt[:, :])
        WB = WBt[:, :].rearrange("p (s k c) -> p s k c", s=NW, k=16)
        P = Prog(nc)

        def mm(out, lhsT, rhs, start, stop, reads, writes):
            P.pe(lambda e: e.matmul(out, lhsT=lhsT, rhs=rhs, start=start, stop=stop), reads, writes)

        def TR(out, in_, reads, writes):
            P.pe(lambda e: e.transpose(out=out, in_=in_, identity=IDF[:, :]), list(reads) + ["IDF"], writes)

        def ACT(out, in_, func, reads, writes, scale=None, bias=None, accum=None):
            kw = {}
            if scale is not None:
                kw["scale"] = scale
            if bias is not None:
                kw["bias"] = bias
            if accum is not None:
                kw["accum_out"] = accum
            P.act(lambda e: e.activation(out=out, in_=in_, func=func, **kw), reads, writes)

        def TT(out, in0, in1, op, reads, writes):
            P.dve(lambda e: e.tensor_tensor(out=out, in0=in0, in1=in1, op=op), reads, writes)

        def TS(out, in0, s1, s2, op0, op1, reads, writes):
            if op1 is None:
                P.dve(lambda e: e.tensor_scalar(out=out, in0=in0, scalar1=s1, scalar2=None, op0=op0), reads, writes)
            else:
                P.dve(lambda e: e.tensor_scalar(out=out, in0=in0, scalar1=s1, scalar2=s2, op0=op0, op1=op1), reads, writes)

        def STT(out, in0, scalar, in1, op0, op1, reads, writes):
            P.dve(lambda e: e.scalar_tensor_tensor(out=out, in0=in0, scalar=scalar, in1=in1, op0=op0, op1=op1), reads, writes)

        def RECIP(out, in_, reads, writes):
            P.dve(lambda e: e.reciprocal(out=out, in_=in_), reads, writes)

        def DMA(eng, out, in_, reads, writes, is_out=False):
            P.dma(eng, lambda e: e.dma_start(out=out, in_=in_), reads, writes, is_out=is_out)

        MUL, ADD, SUB = ALU.mult, ALU.add, ALU.subtract

        DMA("sp", IDF[:, :], dr["idf"][:, :], [], ["IDF"])
        DMA("sp", ONESB[:, :], dr["onesb"][:, :], [], ["ONESB"])
        DMA("sp", RPERM[:, :], dr["rperm"][:, :], [], ["RPERM"])
        ACT(IDB[:, :], IDF[:, :], AF.Copy, ["IDF"], ["IDB"])
        pcol = {"g0": (0, 16), "g1": (16, 16), "retg": (32, 8), "nqg": (40, 1), "nkg": (41, 3),
                "dwb": (44, 8), "cng": (52, 8), "cnb": (60, 8), "dqg": (68, 1), "dkg": (69, 1)}
        for k, (c0, n) in pcol.items():
            DMA("sp", PRM[:, c0:c0 + n], dr[k][:, :], [], ["PRM"])

        def prm(k, j=0, n=1):
            c0 = pcol[k][0] + j
            return PRM[:, c0:c0 + n]

        wlist = []
        for seq in range(nseq):
            for ly in layers:
                nt = 61 if ly == 0 else 64
                for i in range(nt):
                    wlist.append(dr["wl%d" % ly][i])
        wstate = {"next": 0, "loaded": 0}

        def next_w():
            i = wstate["next"]
            wstate["next"] += 1
            upto = min(i + NW - 2, len(wlist) - 1)
            while wstate["loaded"] <= upto:
                j = wstate["loaded"]
                s = j % NW
                DMA("pool", WB[:, s], wlist[j], [], ["WB%d" % s])
                wstate["loaded"] += 1
            s = i % NW
            return WB[:, s], "WB%d" % s

        XNT = None

        def proj_fm(w, tb, M=128):
            wa, wt = w
            ps, pt = PJ.next()
            for kc in range(16):
                mm(ps[0:M, :], wa[:, kc, 0:M], XNT[:, kc, tb * 512:(tb + 1) * 512], kc == 0, kc == 15,
                   [wt, "XNT"], [pt])
            return ps, pt

        def proj_tm(w, tok_aps):
            wa, wt = w
            ps, pt = PJ.next()
            for j, tap in enumerate(tok_aps):
                for kc in range(16):
                    mm(ps[:, j * 128:(j + 1) * 128], tap(kc), wa[:, kc, :], kc == 0, kc == 15, [wt, "XNT"], [pt])
            return ps, pt

        def nat_tok(tt):
            return lambda kc: XNT[:, kc, tt * 128:(tt + 1) * 128]

        def phaseA(xin_d, seq, gname):
            ar.reset(XNT_END)
            XT = [ar.take(D, F32) for _ in range(2)]
            XS = ar.take(D, F32)
            stt = ar.take(4, F32)
            for tt in range(16):
                xt = XT[tt % 2]
                xk = "XT%d" % (tt % 2)
                r0 = seq * S + tt * 128
                DMA("sp", xt, xin_d[r0:r0 + 128, :], [], [xk])
                ACT(XS, xt, AF.Square, [xk], ["XS", "st"], accum=stt[:, 0:1])
                TS(stt[:, 1:2], stt[:, 0:1], 1.0 / D, EPS, MUL, ADD, ["st"], ["st"])
                ACT(stt[:, 1:2], stt[:, 1:2], AF.Sqrt, ["st"], ["st"])
                RECIP(stt[:, 2:3], stt[:, 1:2], ["st"], ["st"])
                ACT(XS, xt, AF.Copy, [xk, "st"], ["XS"], scale=stt[:, 2:3])
                for q in range(4):
                    ps, pt = PJ.next()
                    for j in range(4):
                        kc = 4 * q + j
                        TR(ps[:, j * 128:(j + 1) * 128], XS[:, kc * 128:(kc + 1) * 128], ["XS"], [pt])
                    TT(XNT[:, 4 * q:4 * q + 4, tt * 128:(tt + 1) * 128],
                       ps[:, :].rearrange("p (a b) -> p a b", a=4),
                       prm(gname, 4 * q, 4).unsqueeze(2).to_broadcast([128, 4, 128]), MUL,
                       [pt, "PRM"], ["XNT"])

        def norm_fm(ps, pt, N, gain, out, outtok, SQ, RS):
            ACT(SQ[:, 0:N], ps[:, 0:N], AF.Square, [pt], ["SQ"])
            la, lat = LA.next()
            mm(la[:, 0:N], ONESB[:, :], SQ[:, 0:N], True, True, ["SQ", "ONESB"], [lat])
            TS(RS[:, 0:N], la[:, 0:N], 1.0 / 128, EPS, MUL, ADD, [lat], ["RS"])
            ACT(RS[:, 0:N], RS[:, 0:N], AF.Sqrt, ["RS"], ["RS"])
            RECIP(RS[:, 0:N], RS[:, 0:N], ["RS"], ["RS"])
            STT(out, ps[:, 0:N], gain, RS[:, 0:N], MUL, MUL, [pt, "RS", "PRM"], [outtok])

        def retention(seq, mixd):
            ar.reset(XNT_END)
            QT = ar.take(S, BF16); KT = ar.take(S, BF16); ZS = ar.take(S, BF16)
            VT = ar.take(S, BF16).rearrange("p (t e) -> p t e", t=16)
            RCC = ar.take(S, F32); RSS = ar.take(S, F32)
            RT = [ar.take(5 * 512, F32).rearrange("p (v n) -> p v n", v=5) for _ in range(2)]
            QRAW = ar.take(512, BF16)
            TA = ar.take(512, F32); TB = ar.take(512, F32)
            PTs = [ar.take(512, BF16) for _ in range(3)]
            PTR = Rot([(PTs[i], "PT%d" % i) for i in range(3)])
            SQ = ar.take(512, BF16); RS = ar.take(512, F32)
            MH = [ar.take(S, BF16) for _ in range(2)]
            DMA("sp", RCC, dr["rcc"][:, :], [], ["RCC"])
            DMA("sp", RSS, dr["rss"][:, :], [], ["RSS"])
            for h in range(int(os.environ.get('RET_HEADS', '8'))):
                gam = 1.0 - 2.0 ** (-5.0 - h)
                rt = RT[h % 2]; rtk = "RT%d" % (h % 2)
                DMA("sp", rt, dr["rtab"][h], [], [rtk])
                RSTOP = float(os.environ.get("RET_STOP", "99"))
                if RSTOP <= 1:
                    continue
                for (dst, dtok) in ((QT, "QT"), (KT, "KT")):
                    w = next_w()
                    for tb in range(4):
                        blk = slice(tb * 512, (tb + 1) * 512)
                        if RSTOP <= 1.2:
                            continue
                        ps, pt = proj_fm(w, tb)
                        ACT(QRAW, ps[:, :], AF.Copy, [pt], ["QRAW"])
                        if RSTOP <= 1.5:
                            continue
                        ps2, pt2 = ST.next()
                        mm(ps2[:, :], RPERM[:, :], QRAW, True, True, ["QRAW", "RPERM"], [pt2])
                        if RSTOP <= 1.7:
                            continue
                        if os.environ.get("VARA"):
                            TT(TA, QRAW, RCC[:, blk], MUL, ["QRAW", "RCC"], ["TA"])
                        else:
                            TT(TA, ps[:, :], RCC[:, blk], MUL, [pt, "RCC"], ["TA"])
                        if RSTOP <= 1.8:
                            continue
                        TT(TB, ps2[:, :], RSS[:, blk], MUL, [pt2, "RSS"], ["TB"])
                        if RSTOP <= 1.9:
                            continue
                        TT(dst[:, blk], TA, TB, ADD, ["TA", "TB"], [dtok])
                if RSTOP <= 2:
                    continue
                w = next_w()
                for t4 in range(4):
                    ps, pt = proj_tm(w, [nat_tok(4 * t4 + j) for j in range(4)])
                    ACT(VT[:, 4 * t4:4 * t4 + 4, :], ps[:, :].rearrange("p (a b) -> p a b", a=4), AF.Copy, [pt], ["VT"])
                if RSTOP <= 3:
                    continue
                w = next_w()
                for tb in range(4):
                    ps, pt = proj_fm(w, tb)
                    ACT(ZS[:, tb * 512:(tb + 1) * 512], ps[:, :], AF.Silu, [pt], ["ZS"])
                if RSTOP <= 4:
                    continue
                mh = MH[h % 2]; mhk = "MH%d" % (h % 2)
                for tb in range(4):
                    blk = slice(tb * 512, (tb + 1) * 512)
                    nk = 4 * tb + 4
                    oa, oat = OA.next()

                    def pv(pr, nk=nk, oa=oa, oat=oat):
                        kt, pa, pk = pr
                        mm(oa[:, :], VT[:, kt, :], pa, kt == 0, kt == nk - 1, ["VT", pk], [oat])

                    prev = None
                    for kt in range(nk):
                        st, sk = ST.next()
                        mm(st[:, :], KT[:, kt * 128:(kt + 1) * 128], QT[:, blk], True, True, ["KT", "QT"], [sk])
                        if prev is not None:
                            pv(prev)
                        r = kt - 4 * tb
                        pa, pk = PTR.next()
                        if r < 0:
                            STT(pa, st[:, :], float(gam ** (-128 * r) * SCALE), rt[:, 0, :], MUL, MUL, [sk, rtk], [pk])
                        else:
                            STT(pa, st[:, :], float(SCALE), rt[:, 1 + r, :], MUL, MUL, [sk, rtk], [pk])
                        prev = (kt, pa, pk)
                    pv(prev)
                    ACT(SQ, oa[:, :], AF.Square, [oat], ["SQ"])
                    la, lat = LA.next()
                    mm(la[:, :], ONESB[:, :], SQ, True, True, ["SQ", "ONESB"], [lat])
                    TS(RS, la[:, :], 1.0 / 128, EPS, MUL, ADD, [lat], ["RS"])
                    ACT(RS, RS, AF.Sqrt, ["RS"], ["RS"])
                    RECIP(RS, RS, ["RS"], ["RS"])
                    STT(TA, oa[:, :], prm("retg", h), RS, MUL, MUL, [oat, "RS", "PRM"], ["TA"])
                    TT(mh[:, blk], TA, ZS[:, blk], MUL, ["TA", "ZS"], [mhk])
                DMA("sp", mixd[h], mh, [mhk], ["mixd"])

        def nsa(seq, mixd):
            ar.reset(XNT_END)
            KZ = ar.take(S, BF16)
            VB = ar.take(S, BF16)
            KST = ar.take(S, BF16); KWT = ar.take(S, BF16)
            VS = ar.take(S, BF16).rearrange("p (t e) -> p t e", t=16)
            VW = ar.take(S, BF16).rearrange("p (t e) -> p t e", t=16)
            QN = ar.take(4 * S, BF16).rearrange("p (r n) -> p r n", r=4)
            SG = ar.take(S, BF16)
            W1K = ar.take(4096, BF16).rearrange("p (l e) -> p l e", l=32)
            W1V = ar.take(4096, BF16).rearrange("p (l e) -> p l e", l=32)
            W2K = ar.take(128, BF16); W2V = ar.take(128, BF16)
            POSK = ar.take(32, BF16); POSV = ar.take(32, BF16)
            GG = ar.take(128, BF16); KCN = ar.take(128, BF16); VC = ar.take(128, BF16)
            CM = ar.take(2048, BF16).rearrange("p (r n) -> p r n", r=4)
            WM = ar.take(2048, BF16).rearrange("p (r n) -> p r n", r=4)
            VALID = ar.take(S, BF16)
            OVL = ar.take(32, BF16)
            FTAB = ar.take(512, F32).rearrange("p (t j) -> p t j", t=16)
            EXPD = ar.take(2048, BF16).rearrange("p (t m) -> p t m", t=16)
            SEL = ar.take(24 * 128, BF16).rearrange("p (c m) -> p c m", c=24)
            PTs = [ar.take(512, BF16) for _ in range(4)]
            PTR = Rot([(PTs[i], "PT%d" % i) for i in range(4)])
            PN = ar.take(512, BF16)
            SQ = ar.take(512, BF16); RS = ar.take(512, F32)
            RL = ar.take(512, F32); CF = ar.take(512, F32); ACC = ar.take(512, F32); TMP = ar.take(512, F32)
            U = ar.take(128, F32); U2 = ar.take(128, F32); B1 = ar.take(2, F32)
            IA = ar.take(32, F32); IB = ar.take(32, F32); M8 = ar.take(8, F32); M8b = ar.take(8, F32)
            SELM = ar.take(32, F32)
            MH = [ar.take(S, BF16)] * 2
            for (dst, key, tok) in ((CM, "cm", "CM"), (WM, "wm", "WM"), (EXPD, "expd", "EXPD"), (SEL, "sel", "SEL"),
                                    (FTAB, "ftab", "FTAB")):
                src = dr[key]
                if key in ("expd", "sel"):
                    DMA("sp", dst[0:32], src, [], [tok])
                else:
                    DMA("sp", dst, src, [], [tok])
            DMA("sp", VALID, dr["valid"][:, :], [], ["VALID"])
            DMA("sp", OVL, dr["ovl"][:, :], [], ["OVL"])
            DMA("pool", W1K, dr["w1k"], [], ["W1K"])
            DMA("pool", W1V, dr["w1v"], [], ["W1V"])
            DMA("pool", W2K, dr["w2k"][:, :], [], ["W2K"])
            DMA("pool", W2V, dr["w2v"][:, :], [], ["W2V"])
            DMA("pool", POSK, dr["posk"][:, :], [], ["POSK"])
            DMA("pool", POSV, dr["posv"][:, :], [], ["POSV"])

            def compress(raw, rawtok, W1, w1t, POS, post, W2, w2t, is_k):
                pb, pbt = PJ.next()
                for l in range(32):
                    mm(pb[:, 0:1], W1[:, l, :], POS[:, l:l + 1], l == 0, l == 31, [w1t, post], [pbt])
                ACT(B1[:, 0:1], pb[:, 0:1], AF.Copy, [pbt], ["B1"])
                ph, pht = PJ.next()
                rv = raw.rearrange("p (c s) -> p c s", s=16)
                for l in range(32):
                    rhs = rv[:, 0:127, l] if l < 16 else rv[:, 1:128, l - 16]
                    mm(ph[:, 0:127], W1[:, l, :], rhs, l == 0, l == 31, [w1t, rawtok], [pht])
                u, u2 = U[:, 0:127], U2[:, 0:127]
                TS(u, ph[:, 0:127], B1[:, 0:1], None, ADD, None, [pht, "B1"], ["U"])
                TT(u2, u, u, MUL, ["U"], ["U2"])
                TS(u2, u2, 0.044715, 1.0, MUL, ADD, ["U2"], ["U2"])
                TT(u2, u2, u, MUL, ["U2", "U"], ["U2"])
                ACT(u2, u2, AF.Sigmoid, ["U2"], ["U2"], scale=1.5957691216057308)
                TT(GG[:, 0:127], u, u2, MUL, ["U", "U2"], ["GG"])
                if is_k:
                    pk_, pkt = PJ.next()
                    mm(pk_[:, 0:127], W2, GG[:, 0:127], True, True, [w2t, "GG"], [pkt])
                    norm_fm(pk_, pkt, 127, prm("nkg", 0), KCN[:, 0:127], "KCN", SQ, RS)
                else:
                    pv_, pvt = PJ.next()
                    mm(pv_[0:127, 0:128], GG[:, 0:127], W2, True, True, [w2t, "GG"], [pvt])
                    ACT(VC[0:127, :], pv_[0:127, 0:128], AF.Copy, [pvt], ["VC"])

            def attn(qap, tiles, oa, oat, la, lat):
                n = len(tiles)

                def pv(pr):
                    i, t, pa, pk = pr
                    nk = t["nk"]
                    mm(oa[:, :], t["v"][0], pa[0:nk, :], i == 0, i == n - 1, [t["v"][1], pk], [oat])
                    mm(la[:, :], ONESB[0:nk, :], pa[0:nk, :], i == 0, i == n - 1, ["ONESB", pk], [lat])

                prev = None
                for i, t in enumerate(tiles):
                    st, sk = ST.next()
                    nk = t["nk"]
                    b = t.get("bias")
                    mm(st[0:nk, :], t["k"][0], qap, True, b is None, [t["k"][1], "QN"], [sk])
                    if b is not None:
                        mm(st[0:nk, :], b[0], b[1], False, True, ["EXPD", "VB"], [sk])
                    if prev is not None:
                        pv(prev)
                    pa, pk = PTR.next()
                    ACT(pa[0:nk, :], st[0:nk, :], AF.Exp, [sk], [pk], scale=float(SCALE))
                    m = t.get("mask")
                    if m is not None:
                        TT(pa[0:nk, :], pa[0:nk, :], m[0], MUL, [pk, m[1]], [pk])
                    prev = (i, t, pa, pk)
                pv(prev)

            def branch_fin(oa, oat, la, lat, gcol, blk, first):
                pg, pgt = PJ.next()
                mm(pg[:, :], SEL[0:32, gcol, :], SG[0:32, blk], True, True, ["SEL", "SG"], [pgt])
                TS(RL, la[:, :], 1e-30, None, ADD, None, [lat], ["RL"])
                RECIP(RL, RL, ["RL"], ["RL"])
                TT(CF, RL, pg[:, :], MUL, ["RL", pgt], ["CF"])
                if first:
                    TT(ACC, oa[:, :], CF, MUL, [oat, "CF"], ["ACC"])
                else:
                    TT(TMP, oa[:, :], CF, MUL, [oat, "CF"], ["TMP"])
                    TT(ACC, ACC, TMP, ADD, ["ACC", "TMP"], ["ACC"])

            for g in range(2):
                w = next_w()
                for tb in range(4):
                    ps, pt = proj_fm(w, tb)
                    ACT(KZ[:, tb * 512:(tb + 1) * 512], ps[:, :], AF.Copy, [pt], ["KZ"])
                w = next_w()
                for tb in range(4):
                    ps, pt = proj_fm(w, tb)
                    ACT(VB[:, tb * 512:(tb + 1) * 512], ps[:, :], AF.Copy, [pt], ["VB"])
                w = next_w()
                for tb in range(4):
                    ps, pt = proj_fm(w, tb)
                    norm_fm(ps, pt, 512, prm("nkg", 1), KST[:, tb * 512:(tb + 1) * 512], "KST", SQ, RS)
                w = next_w()
                for t4 in range(4):
                    ps, pt = proj_tm(w, [nat_tok(4 * t4 + j) for j in range(4)])
                    ACT(VS[:, 4 * t4:4 * t4 + 4, :], ps[:, :].rearrange("p (a b) -> p a b", a=4), AF.Copy, [pt], ["VS"])
                w = next_w()
                for tb in range(4):
                    ps, pt = proj_fm(w, tb)
                    norm_fm(ps, pt, 512, prm("nkg", 2), KWT[:, tb * 512:(tb + 1) * 512], "KWT", SQ, RS)
                w = next_w()
                for t4 in range(4):
                    ps, pt = proj_tm(w, [nat_tok(4 * t4 + j) for j in range(4)])
                    ACT(VW[:, 4 * t4:4 * t4 + 4, :], ps[:, :].rearrange("p (a b) -> p a b", a=4), AF.Copy, [pt], ["VW"])
                if g == 0:
                    w = next_w()
                    for tb in range(4):
                        ps, pt = proj_fm(w, tb, M=32)
                        ACT(SG[0:32, tb * 512:(tb + 1) * 512], ps[0:32, :], AF.Sigmoid, [pt], ["SG"])
                compress(KZ, "KZ", W1K, "W1K", POSK, "POSK", W2K, "W2K", True)
                compress(VB, "VB", W1V, "W1V", POSV, "POSV", W2V, "W2V", False)
                for r in range(4):
                    w = next_w()
                    for tb in range(4):
                        ps, pt = proj_fm(w, tb)
                        norm_fm(ps, pt, 512, prm("nqg", 0), QN[:, r, tb * 512:(tb + 1) * 512], "QN", SQ, RS)
                for tb in (2, 3):
                    blk = slice(tb * 512, (tb + 1) * 512)
                    pim, pimt = PJ.next()
                    pns = []
                    for r in range(4):
                        st, sk = ST.next()
                        mm(st[0:127, :], KCN[:, 0:127], QN[:, r, blk], True, True, ["KCN", "QN"], [sk])
                        pa, pk = PTR.next()
                        ACT(pa[0:127, :], st[0:127, :], AF.Exp, [sk], [pk], scale=float(SCALE))
                        TT(pa[0:127, :], pa[0:127, :], VALID[0:127, blk], MUL, [pk, "VALID"], [pk])
                        la, lat = LA.next()
                        mm(la[:, :], ONESB[0:127, :], pa[0:127, :], True, True, ["ONESB", pk], [lat])
                        TS(RL, la[:, :], 1e-30, None, ADD, None, [lat], ["RL"])
                        RECIP(RL, RL, ["RL"], ["RL"])
                        TT(pa[0:127, :], pa[0:127, :], RL[0:127, :], MUL, [pk, "RL"], [pk])
                        pns.append((pa, pk))
                    for j in range(4):
                        for r in range(4):
                            pa, pk = pns[r]
                            mm(pim[:, j * 32:(j + 1) * 32], pa[0:127, j * 128:(j + 1) * 128], OVL[0:127, :],
                               r == 0, r == 3, [pk, "OVL"], [pimt])
                    for j in range(4):
                        tt = 4 * tb + j
                        TT(IA, pim[:, j * 32:(j + 1) * 32], FTAB[:, tt, :], ADD, [pimt, "FTAB"], ["IA"])
                        P.dve(lambda e: e.max(out=M8, in_=IA), ["IA"], ["M8"])
                        P.dve(lambda e: e.match_replace(out=IB, in_to_replace=M8, in_values=IA, imm_value=-3.0e38),
                              ["IA", "M8"], ["IB"])
                        P.dve(lambda e: e.max(out=M8b, in_=IB), ["IB"], ["M8b"])
                        TS(SELM, IA, M8b[:, 7:8], 1.0, ALU.is_ge, SUB, ["IA", "M8b"], ["SELM"])
                        ptp, ptpt = PJ.next()
                        TR(ptp[0:32, 0:128], SELM, ["SELM"], [ptpt])
                        ACT(VB[0:32, tt * 128:(tt + 1) * 128], ptp[0:32, 0:128], AF.Copy, [ptpt], ["VB"])
                for r in range(4):
                    h = 4 * g + r
                    w = next_w()
                    for tb in range(4):
                        ps, pt = proj_fm(w, tb)
                        ACT(KZ[:, tb * 512:(tb + 1) * 512], ps[:, :], AF.Silu, [pt], ["KZ"])
                    mh = MH[0]; mhk = "MH0"
                    for tb in range(4):
                        blk = slice(tb * 512, (tb + 1) * 512)
                        qap = QN[:, r, blk]
                        oa, oat = OA.next(); la, lat = LA.next()
                        attn(qap, [dict(k=(KCN[:, 0:127], "KCN"), nk=127, v=(VC[0:127, :], "VC"),
                                        mask=(VALID[0:127, blk], "VALID"))], oa, oat, la, lat)
                        branch_fin(oa, oat, la, lat, 0 * 8 + h, blk, True)
                        tiles = []
                        for kt in range(4 * tb + 4):
                            t = dict(k=(KST[:, kt * 128:(kt + 1) * 128], "KST"), nk=128, v=(VS[:, kt, :], "VS"))
                            if tb >= 2:
                                t["bias"] = (EXPD[0:32, kt, :], VB[0:32, blk])
                            rr = kt - 4 * tb
                            if rr >= 0:
                                t["mask"] = (CM[:, rr, :], "CM")
                            tiles.append(t)
                        oa, oat = OA.next(); la, lat = LA.next()
                        attn(qap, tiles, oa, oat, la, lat)
                        branch_fin(oa, oat, la, lat, 1 * 8 + h, blk, False)
                        tiles = []
                        for kt in range(max(0, 4 * tb - 4), 4 * tb + 4):
                            rr = kt - 4 * tb
                            m = (CM[:, rr, :], "CM") if rr >= 0 else (WM[:, rr + 4, :], "WM")
                            tiles.append(dict(k=(KWT[:, kt * 128:(kt + 1) * 128], "KWT"), nk=128, v=(VW[:, kt, :], "VW"), mask=m))
                        oa, oat = OA.next(); la, lat = LA.next()
                        attn(qap, tiles, oa, oat, la, lat)
                        branch_fin(oa, oat, la, lat, 2 * 8 + h, blk, False)
                        TT(mh[:, blk], ACC, KZ[:, blk], MUL, ["ACC", "KZ"], [mhk])
                    DMA("sp", mixd[8 + h], mh, [mhk], ["mixd"])

        def conv_branch(seq, mixd):
            ar.reset(XNT_END)
            A = ar.take(8 * S, F32).rearrange("p (j n) -> p j n", j=8)
            A0 = ar.take(S + 32, BF16)
            DG = ar.take(31 * 128, BF16).rearrange("p (k m) -> p k m", k=31)
            DWW = ar.take(8 * 31, F32).rearrange("p (j k) -> p j k", j=8)
            SGT = ar.take(512, F32)
            MU = ar.take(S, F32); RSTD = ar.take(S, F32)
            TBF = [ar.take(512, BF16) for _ in range(2)]
            TSQ = [ar.take(512, BF16) for _ in range(2)]
            TV = SGT; T1 = ar.take(512, F32); T2 = ar.take(512, F32)
            ZSb = ar.take(512, BF16)
            MH = [ar.take(S, BF16)] * 2
            DMA("sp", DWW, dr["dww"], [], ["DWW"])
            P.dve(lambda e: e.memset(A0[:, 0:32], 0.0), [], ["A0"])
            for j in range(8):
                wu = next_w(); wg = next_w()
                for k in range(31):
                    TS(DG[:, k, :], IDB[:, :], DWW[:, j, k:k + 1], None, MUL, None, ["IDB", "DWW"], ["DG"])
                for tb in range(4):
                    pu, put = proj_fm(wu, tb)
                    pg, pgt = proj_fm(wg, tb)
                    ACT(SGT, pg[:, :], AF.Sigmoid, [pgt], ["SGT"])
                    TT(A0[:, 32 + tb * 512:32 + (tb + 1) * 512], pu[:, :], SGT, MUL, [put, "SGT"], ["A0"])
                for tb in range(4):
                    pc, pct = ST.next()
                    for k in range(31):
                        mm(pc[:, :], DG[:, k, :], A0[:, 2 + k + tb * 512:2 + k + (tb + 1) * 512], k == 0, k == 30,
                           ["DG", "A0"], [pct])
                    ACT(A[:, j, tb * 512:(tb + 1) * 512], pc[:, :], AF.Identity, [pct, "PRM"], ["A"], bias=prm("dwb", j))
            for tb in range(4):
                blk = slice(tb * 512, (tb + 1) * 512)
                s1, s1t = LA.next(); s2, s2t = OA.next()
                for j in range(8):
                    ACT(TBF[j % 2], A[:, j, blk], AF.Copy, ["A"], ["TBF%d" % (j % 2)])
                    ACT(TSQ[j % 2], A[:, j, blk], AF.Square, ["A"], ["TSQ%d" % (j % 2)])
                    mm(s1[:, :], ONESB[:, :], TBF[j % 2], j == 0, j == 7, ["ONESB", "TBF%d" % (j % 2)], [s1t])
                    mm(s2[:, :], ONESB[:, :], TSQ[j % 2], j == 0, j == 7, ["ONESB", "TSQ%d" % (j % 2)], [s2t])
                TS(MU[:, blk], s1[:, :], 1.0 / 1024, None, MUL, None, [s1t], ["MU"])
                TT(TV, MU[:, blk], MU[:, blk], MUL, ["MU"], ["SGT"])
                STT(TV, s2[:, :], 1.0 / 1024, TV, MUL, SUB, [s2t, "SGT"], ["SGT"])
                TS(TV, TV, EPS, None, ADD, None, ["SGT"], ["SGT"])
                ACT(TV, TV, AF.Sqrt, ["SGT"], ["SGT"])
                RECIP(RSTD[:, blk], TV, ["SGT"], ["RSTD"])
            for j in range(8):
                wz = next_w()
                mh = MH[0]; mhk = "MH0"
                for tb in range(4):
                    blk = slice(tb * 512, (tb + 1) * 512)
                    pz, pzt = proj_fm(wz, tb)
                    ACT(ZSb, pz[:, :], AF.Silu, [pzt], ["ZSb"])
                    TT(T1, A[:, j, blk], MU[:, blk], SUB, ["A", "MU"], ["T1"])
                    TT(T1, T1, RSTD[:, blk], MUL, ["T1", "RSTD"], ["T1"])
                    ACT(T2, T1, AF.Silu, ["T1", "PRM"], ["T2"], scale=prm("cng", j), bias=prm("cnb", j))
                    TT(mh[:, blk], T2, ZSb, MUL, ["T2", "ZSb"], [mhk])
                DMA("sp", mixd[j], mh, [mhk], ["mixd"])

        def dilated(seq, mixd):
            ar.reset(XNT_END)
            QN = ar.take(S, BF16); KN = ar.take(S, BF16)
            VD = ar.take(S, BF16).rearrange("p (t e) -> p t e", t=16)
            OACC = ar.take(S, F32); LACC = ar.take(S, F32)
            DM = ar.take(1024, BF16).rearrange("p (v n) -> p v n", v=2)
            PTs = [ar.take(512, BF16) for _ in range(3)]
            PTR = Rot([(PTs[i], "PT%d" % i) for i in range(3)])
            SQ = ar.take(512, BF16); RS = ar.take(512, F32)
            RL = ar.take(512, F32); T1 = ar.take(512, F32); ZSb = ar.take(512, BF16)
            MH = [ar.take(S, BF16) for _ in range(2)]
            DMA("sp", DM, dr["dm"], [], ["DM"])
            for hh in range(4):
                for gi, dil in enumerate((1, 4, 16)):
                    L = S // dil
                    nqt = L // 128

                    def sub(buf, c, qt, dil=dil):
                        return buf.rearrange("p (a d) -> p a d", d=dil)[:, 128 * qt:128 * qt + 128, c]

                    w = next_w()
                    for tb in range(4):
                        ps, pt = proj_fm(w, tb)
                        norm_fm(ps, pt, 512, prm("dqg", 0), QN[:, tb * 512:(tb + 1) * 512], "QN", SQ, RS)
                    w = next_w()
                    for tb in range(4):
                        ps, pt = proj_fm(w, tb)
                        norm_fm(ps, pt, 512, prm("dkg", 0), KN[:, tb * 512:(tb + 1) * 512], "KN", SQ, RS)
                    w = next_w()
                    QL = [(c, qt) for c in range(dil) for qt in range(nqt)]
                    for t4 in range(4):
                        taps = []
                        for j in range(4):
                            c, qt = QL[4 * t4 + j]
                            taps.append(lambda kc, c=c, qt=qt: sub(XNT[:, kc, :], c, qt))
                        ps, pt = proj_tm(w, taps)
                        ACT(VD[:, 4 * t4:4 * t4 + 4, :], ps[:, :].rearrange("p (a b) -> p a b", a=4), AF.Copy, [pt], ["VD"])
                    for q4 in range(4):
                        oa, oat = OA.next(); la, lat = LA.next()
                        for half in range(2):
                            st, sk = ST.next()
                            info = []
                            for u in range(2):
                                ti = 4 * q4 + 2 * half + u
                                c, qt = QL[ti]
                                tp = ti - 1 if qt > 0 else ti
                                cp, qp = QL[tp]
                                qap = sub(QN, c, qt)
                                mm(st[:, u * 256:u * 256 + 128], sub(KN, cp, qp), qap, True, True, ["KN", "QN"], [sk])
                                mm(st[:, u * 256 + 128:u * 256 + 256], sub(KN, c, qt), qap, True, True, ["KN", "QN"], [sk])
                                info.append((ti, tp, qt))
                            pa, pk = PTR.next()
                            ACT(pa, st[:, :], AF.Exp, [sk], [pk], scale=float(SCALE))
                            for u in range(2):
                                ti, tp, qt = info[u]
                                v = 0 if qt > 0 else 1
                                TT(pa[:, u * 256:u * 256 + 256], pa[:, u * 256:u * 256 + 256], DM[:, v, 0:256] if v == 0 else DM[:, 1, 0:256],
                                   MUL, [pk, "DM"], [pk])
                            for u in range(2):
                                ti, tp, qt = info[u]
                                col = (2 * half + u) * 128
                                mm(oa[:, col:col + 128], VD[:, tp, :], pa[:, u * 256:u * 256 + 128], True, False, ["VD", pk], [oat])
                                mm(oa[:, col:col + 128], VD[:, ti, :], pa[:, u * 256 + 128:u * 256 + 256], False, True, ["VD", pk], [oat])
                                mm(la[:, col:col + 128], ONESB[:, :], pa[:, u * 256:u * 256 + 128], True, False, ["ONESB", pk], [lat])
                                mm(la[:, col:col + 128], ONESB[:, :], pa[:, u * 256 + 128:u * 256 + 256], False, True, ["ONESB", pk], [lat])
                        if nqt >= 4:
                            c = (4 * q4) // nqt
                            qt0 = (4 * q4) % nqt

                            def dst(buf, c=c, qt0=qt0, dil=dil):
                                return buf.rearrange("p (a d) -> p a d", d=dil)[:, 128 * qt0:128 * qt0 + 512, c]

                            srcv = lambda p_: p_[:, :]
                        else:
                            def dst(buf, q4=q4):
                                return buf.rearrange("p (a d) -> p d a", d=16)[:, 4 * q4:4 * q4 + 4, :]

                            srcv = lambda p_: p_[:, :].rearrange("p (u i) -> p u i", u=4)
                        if gi == 0:
                            ACT(dst(OACC), srcv(oa), AF.Copy, [oat], ["OACC"])
                            ACT(dst(LACC), srcv(la), AF.Copy, [lat], ["LACC"])
                        else:
                            TT(dst(OACC), srcv(oa), dst(OACC), ADD, [oat, "OACC"], ["OACC"])
                            TT(dst(LACC), srcv(la), dst(LACC), ADD, [lat, "LACC"], ["LACC"])
                w = next_w()
                mh = MH[hh % 2]; mhk = "MH%d" % (hh % 2)
                for tb in range(4):
                    blk = slice(tb * 512, (tb + 1) * 512)
                    pz, pzt = proj_fm(w, tb)
                    ACT(ZSb, pz[:, :], AF.Silu, [pzt], ["ZSb"])
                    RECIP(RL, LACC[:, blk], ["LACC"], ["RL"])
                    TT(T1, OACC[:, blk], RL, MUL, ["OACC", "RL"], ["T1"])
                    TT(mh[:, blk], T1, ZSb, MUL, ["T1", "ZSb"], [mhk])
                DMA("sp", mixd[8 + hh], mh, [mhk], ["mixd"])

        def phaseC(seq, xin_d, xout_d, mixd, wo, nmc, is_out):
            ar.reset(0)
            WO = ar.take(4 * nmc * 512, BF16).rearrange("p (b c n) -> p b c n", b=4, c=nmc)
            XR = [ar.take(D, F32) for _ in range(2)]
            Y = [ar.take(D, F32) for _ in range(2)]
            MT = [ar.take(nmc * 128, BF16).rearrange("p (c t) -> p c t", c=nmc) for _ in range(2)]
            for nb in range(4):
                DMA("pool", WO[:, nb], wo[nb], [], ["WO"])
            for tt in range(16):
                b = tt % 2
                r0 = seq * S + tt * 128
                DMA("sp", MT[b], mixd[0:nmc, :, tt * 128:(tt + 1) * 128].rearrange("c p t -> p c t"), ["mixd"], ["MT%d" % b])
                DMA("sp", XR[b], xin_d[r0:r0 + 128, :], [], ["XR%d" % b])
                for nb in range(4):
                    ps, pt = PJ.next()
                    for mc in range(nmc):
                        mm(ps[:, :], MT[b][:, mc, :], WO[:, nb, mc, :], mc == 0, mc == nmc - 1, ["MT%d" % b, "WO"], [pt])
                    TT(Y[b][:, nb * 512:(nb + 1) * 512], ps[:, :], XR[b][:, nb * 512:(nb + 1) * 512], ADD,
                       [pt, "XR%d" % b], ["Y%d" % b])
                DMA("sp", xout_d[r0:r0 + 128, :], Y[b], ["Y%d" % b], ["xout"], is_out=is_out)

        XNT = ar.take(16 * S, BF16).rearrange("p (k n) -> p k n", k=16)
        nl = len(layers)
        for seq in range(nseq):
            for li, ly in enumerate(layers):
                xin = x_d if li == 0 else x1_d
                xout = out_d if li == nl - 1 else x1_d
                mixd = mix_d[seq * 2 + li]
                on = lambda nm: phases is None or nm in phases
                P.fence()
                if on("A"):
                    phaseA(xin, seq, "g%d" % ly)
                P.fence()
                if ly == 0:
                    if on("ret"):
                        retention(seq, mixd)
                    else:
                        wstate["next"] += 32; wstate["loaded"] = max(wstate["loaded"], wstate["next"])
                    P.fence()
                    if on("nsa"):
                        nsa(seq, mixd)
                else:
                    if on("conv"):
                        conv_branch(seq, mixd)
                    else:
                        wstate["next"] += 24; wstate["loaded"] = max(wstate["loaded"], wstate["next"])
                    P.fence()
                    if on("dil"):
                        dilated(seq, mixd)
                P.fence()
                if on("C"):
                    phaseC(seq, xin, xout, mixd, dr["wo%d" % ly], 16 if ly == 0 else 12, li == nl - 1)
        P.emit()
    return nc


_CACHE = {}


def _prep(inputs, nseq, ncores):
    consts = host_consts()
    params = host_params(inputs)
    x = np.asarray(inputs["x"], np.float32)
    maps = []
    for c in range(ncores):
        m = {"x": np.ascontiguousarray(x[c * nseq:(c + 1) * nseq].reshape(nseq * S, D))}
        for k, v in consts.items():
            m["c_" + k] = v
        for k, v in params.items():
            m["p_" + k] = v
        maps.append(m)
    return maps


def kernel(**inputs):
    ncores, nseq = 8, 2
    if "nc" not in _CACHE:
        _CACHE["nc"] = build(nseq=nseq, layers=(0, 1))
    nc = _CACHE["nc"]
    maps = _prep(inputs, nseq, ncores)
    res = run_bass_kernel_spmd(nc, maps, core_ids=list(range(ncores)))
    outs = [np.asarray(r["out"], np.float32).reshape(nseq, S, D) for r in res.results]
    return np.concatenate(outs, axis=0)
```

```python
import math
import numpy as np
import ml_dtypes
import concourse.bass as bass
import concourse.mybir as mybir
from concourse.bass_utils import run_bass_kernel_spmd
from contextlib import ExitStack

F32 = mybir.dt.float32
BF16 = mybir.dt.bfloat16
AF = mybir.ActivationFunctionType
ALU = mybir.AluOpType

S = 2048
D = 2048
EPS = 1e-6
SCALE = 128 ** -0.5
NEGB = 30000.0


class _Op:
    __slots__ = ("eng", "fn", "deps", "dma", "sig", "val", "sem", "slot")


class Prog:
    ENGS = ("pe", "act", "dve", "pool", "sp")
    NSLOT = 8

    def __init__(self, nc):
        self.nc = nc
        self.ops = []
        self.last_w = {}
        self.readers = {}
        self.out_dmas = []
        self.last_eng = {}
        self.last_slot = {}
        self.dcnt = {e: 0 for e in self.ENGS}
        self.pending = {}

    def fence(self):
        snap = list(self.last_eng.values()) + list(self.last_slot.values())
        for e in self.ENGS:
            self.pending[e] = list(snap)
        self.last_w = {}
        self.readers = {}

    def add(self, eng, fn, reads=(), writes=(), dma=False, is_out=False):
        op = _Op()
        idx = len(self.ops)
        op.eng, op.fn, op.dma = eng, fn, dma
        op.sig = dma
        op.val = 0
        op.sem = None
        op.slot = None
        ex = [t for t in reads if t[:2] in ("PJ", "ST", "OA", "LA")]
        if ex:
            reads = [t for t in reads if t not in ex]
            writes = list(writes) + ex
        deps = set()
        for t in reads:
            w = self.last_w.get(t)
            if w is not None:
                deps.add(w)
        for t in writes:
            w = self.last_w.get(t)
            if w is not None:
                deps.add(w)
            for r in self.readers.get(t, ()):
                deps.add(r)
        pf = self.pending.pop(eng, None)
        if pf:
            deps.update(pf)
        for t in reads:
            self.readers.setdefault(t, []).append(idx)
        for t in writes:
            self.last_w[t] = idx
            self.readers[t] = []
        if dma:
            k = self.dcnt[eng]
            self.dcnt[eng] += 1
            s = k % self.NSLOT
            op.slot = (eng, s, 16 * (k // self.NSLOT + 1))
            prev = self.last_slot.get((eng, s))
            if prev is not None:
                deps.add(prev)
            self.last_slot[(eng, s)] = idx
        keep = []
        for d in deps:
            o = self.ops[d]
            if eng == "pe" and o.eng == "pe" and not o.dma and not dma:
                continue
            keep.append(d)
            o.sig = True
        op.deps = keep
        self.ops.append(op)
        if not dma:
            self.last_eng[eng] = idx
        if is_out:
            self.out_dmas.append(idx)
        return idx

    def pe(self, fn, reads=(), writes=()):
        return self.add("pe", fn, reads, writes)

    def act(self, fn, reads=(), writes=()):
        return self.add("act", fn, reads, writes)

    def dve(self, fn, reads=(), writes=()):
        return self.add("dve", fn, reads, writes)

    def pool(self, fn, reads=(), writes=()):
        return self.add("pool", fn, reads, writes)

    def dma(self, eng, fn, reads=(), writes=(), is_out=False):
        return self.add(eng, fn, reads, writes, dma=True, is_out=is_out)

    def emit(self):
        nc = self.nc
        with ExitStack() as es:
            esem = {e: es.enter_context(nc.semaphore("s_" + e)) for e in self.ENGS if e != "sp"}
            dsem = {e: [es.enter_context(nc.semaphore("d_%s%d" % (e, i))) for i in range(self.NSLOT)]
                    for e in ("sp", "pool", "act")}
            cnt = {e: 0 for e in self.ENGS}
            for op in self.ops:
                if op.dma:
                    e, s, v = op.slot
                    op.sem = dsem[e][s]
                    op.val = v
                elif op.sig:
                    cnt[op.eng] += 1
                    op.sem = esem[op.eng]
                    op.val = cnt[op.eng]
            per_eng = {e: [] for e in self.ENGS}
            for i, op in enumerate(self.ops):
                per_eng[op.eng].append(i)
            ops = self.ops
            out_dmas = self.out_dmas
            block = es.enter_context(nc.Block())

            def run(eng_name, h):
                waited = {}
                for i in per_eng[eng_name]:
                    op = ops[i]
                    need = {}
                    for d in op.deps:
                        o = ops[d]
                        key = id(o.sem)
                        if waited.get(key, 0) >= o.val:
                            continue
                        if key not in need or need[key][1] < o.val:
                            need[key] = (o.sem, o.val)
                    for key, (sem, val) in need.items():
                        h.wait_ge(sem, val)
                        waited[key] = val
                    ins = op.fn(h)
                    if op.sig:
                        ins.then_inc(op.sem, 16 if op.dma else 1)
                if eng_name == "sp":
                    fin = {}
                    for i in out_dmas:
                        o = ops[i]
                        key = id(o.sem)
                        if key not in fin or fin[key][1] < o.val:
                            fin[key] = (o.sem, o.val)
                    for key, (sem, val) in fin.items():
                        h.wait_ge(sem, val)

            @block.tensor
            def _(e):
                run("pe", e)

            @block.scalar
            def _(e):
                run("act", e)

            @block.vector
            def _(e):
                run("dve", e)

            @block.gpsimd
            def _(e):
                run("pool", e)

            @block.sync
            def _(e):
                run("sp", e)


class Rot:
    def __init__(self, items):
        self.items = items
        self.i = 0

    def next(self):
        it = self.items[self.i % len(self.items)]
        self.i += 1
        return it


def l0_cols():
    cols = []
    for h in range(8):
        cols += [128 * h, 1024 + 128 * h, 2048 + 128 * h, 3072 + 128 * h]
    for g in range(2):
        cols += [5120 + 128 * g, 5376 + 128 * g, 5632 + 128 * g, 5888 + 128 * g,
                 6144 + 128 * g, 6400 + 128 * g]
        if g == 0:
            cols += [-1]
        cols += [4096 + 128 * (4 * g + r) for r in range(4)]
        cols += [6680 + 128 * (4 * g + r) for r in range(4)]
    return cols


def l1_cols():
    cols = []
    for j in range(8):
        cols += [128 * j, 1024 + 128 * j]
    for j in range(8):
        cols += [2048 + 128 * j]
    for hh in range(4):
        for gi in range(3):
            hd = 4 * gi + hh
            cols += [3072 + 128 * hd, 3072 + 1536 + 128 * hd, 3072 + 3072 + 128 * hd]
        cols += [3072 + 4608 + 128 * hh]
    return cols


def tile_w(w, cols):
    K = w.shape[0]
    out = np.zeros((len(cols), 128, K // 128, 128), np.float32)
    for i, c in enumerate(cols):
        if c < 0:
            blk = np.zeros((K, 128), np.float32)
            blk[:, :24] = w[:, 6656:6680]
        else:
            blk = w[:, c:c + 128]
        out[i] = blk.reshape(K // 128, 128, 128).transpose(1, 0, 2)
    return out


def host_consts():
    c = {}
    bf = ml_dtypes.bfloat16
    c["idf"] = np.eye(128, dtype=np.float32)
    c["onesb"] = np.ones((128, 128), bf)
    rp = np.zeros((128, 128), np.float32)
    for m in range(128):
        rp[(m + 64) % 128, m] = 1.0
    c["rperm"] = rp.astype(bf)
    half = 64
    inv = (10000.0 ** (-np.arange(half, dtype=np.float32) / half)).astype(np.float32)
    ang = (np.arange(S, dtype=np.float32)[None, :] * inv[:, None]).astype(np.float32)
    cos, sin = np.cos(ang), np.sin(ang)
    c["rcc"] = np.concatenate([cos, cos], 0).astype(np.float32)
    c["rss"] = np.concatenate([-sin, sin], 0).astype(np.float32)
    i = np.arange(128, dtype=np.float64)[:, None]
    j = np.arange(512, dtype=np.float64)[None, :]
    rt = np.zeros((8, 128, 5, 512), np.float32)
    for h in range(8):
        lg = math.log(1.0 - 2.0 ** (-5.0 - h))
        rt[h, :, 0, :] = np.exp((j - i) * lg)
        for r in range(4):
            e = j - i - 128 * r
            rt[h, :, 1 + r, :] = np.where(e >= 0, np.exp(np.maximum(e, 0) * lg), 0.0)
    c["rtab"] = rt
    cm = np.zeros((128, 4, 512), np.float32)
    wm = np.zeros((128, 4, 512), np.float32)
    for r in range(4):
        cm[:, r, :] = (j - i - 128 * r >= 0)
        wm[:, r, :] = (j - i - 128 * (r - 4) <= 511)
    c["cm"] = cm.astype(bf)
    c["wm"] = wm.astype(bf)
    n = np.arange(S)[None, :]
    cc = np.arange(128)[:, None]
    valid = ((16 * cc + 31 <= n) & (cc < 127)).astype(np.float32)
    c["valid"] = valid.astype(bf)
    cs = np.arange(127) * 16
    ss = np.arange(32) * 64
    ovl = np.zeros((128, 32), np.float32)
    ovl[:127] = ((cs[:, None] < ss[None, :] + 64) & (cs[:, None] + 32 > ss[None, :]))
    c["ovl"] = ovl.astype(bf)
    t = np.arange(S)
    cur = t // 64
    jj = np.arange(32)
    forced = (jj[None, :] == 0) | (jj[None, :] == cur[:, None]) | (jj[None, :] == cur[:, None] - 1)
    future = jj[None, :] > cur[:, None]
    ft = np.where(forced, 1e30, np.where(future, -1e30, 0.0)).astype(np.float32)
    c["ftab"] = np.ascontiguousarray(ft.reshape(16, 128, 32).transpose(1, 0, 2))
    ex = np.zeros((32, 16, 128), np.float32)
    for kt in range(16):
        for m in range(128):
            ex[(128 * kt + m) // 64, kt, m] = NEGB
    c["expd"] = ex.astype(bf)
    sel = np.zeros((32, 24, 128), np.float32)
    for k in range(24):
        sel[k, k, :] = 1.0
    c["sel"] = sel.astype(bf)
    m_ = np.arange(128)[:, None]
    n_ = np.arange(128)[None, :]
    prev = (m_ >= n_).astype(np.float32)
    diag = (m_ <= n_).astype(np.float32)
    dm = np.zeros((128, 2, 512), np.float32)
    dm[:, 0, :] = np.concatenate([prev, diag, prev, diag], 1)
    dm[:, 1, :] = np.concatenate([np.zeros_like(prev), diag, prev, diag], 1)
    c["dm"] = dm.astype(bf)
    return c


CONST_SPECS = {
    "idf": ([128, 128], F32), "onesb": ([128, 128], BF16), "rperm": ([128, 128], BF16),
    "rcc": ([128, S], F32), "rss": ([128, S], F32), "rtab": ([8, 128, 5, 512], F32),
    "cm": ([128, 4, 512], BF16), "wm": ([128, 4, 512], BF16), "valid": ([128, S], BF16),
    "ovl": ([128, 32], BF16), "ftab": ([128, 16, 32], F32), "expd": ([32, 16, 128], BF16),
    "sel": ([32, 24, 128], BF16), "dm": ([128, 2, 512], BF16),
}


def host_params(inp):
    p = {}
    f = np.float32
    p["wl0"] = tile_w(np.asarray(inp["ev_w_in"][0], f), l0_cols())
    p["wl1"] = tile_w(np.asarray(inp["od_w_in"][0], f), l1_cols())

    def wo_l(w):
        K = w.shape[0]
        return np.ascontiguousarray(w.reshape(K // 128, 128, 4, 512).transpose(2, 1, 0, 3))

    p["wo0"] = wo_l(np.asarray(inp["ev_w_out"][0], f))
    p["wo1"] = wo_l(np.asarray(inp["od_w_out"][0], f))
    p["g0"] = np.ascontiguousarray(np.asarray(inp["ev_norm"][0], f).reshape(16, 128).T)
    p["g1"] = np.ascontiguousarray(np.asarray(inp["od_norm"][0], f).reshape(16, 128).T)
    p["retg"] = np.ascontiguousarray(np.asarray(inp["ev_ret_norm"][0], f).T)
    p["nqg"] = np.ascontiguousarray(np.asarray(inp["ev_nsa_q_norm"][0], f).reshape(128, 1))
    p["nkg"] = np.ascontiguousarray(np.asarray(inp["ev_nsa_k_norm"][0], f).T)
    p["posk"] = np.ascontiguousarray(np.asarray(inp["ev_cmp_pos_k"][0], f).T)
    p["posv"] = np.ascontiguousarray(np.asarray(inp["ev_cmp_pos_v"][0], f).T)
    p["w1k"] = np.ascontiguousarray(np.asarray(inp["ev_cmp_k_w1"][0], f).reshape(32, 128, 128).transpose(1, 0, 2))
    p["w1v"] = np.ascontiguousarray(np.asarray(inp["ev_cmp_v_w1"][0], f).reshape(32, 128, 128).transpose(1, 0, 2))
    p["w2k"] = np.ascontiguousarray(np.asarray(inp["ev_cmp_k_w2"][0], f))
    p["w2v"] = np.ascontiguousarray(np.asarray(inp["ev_cmp_v_w2"][0], f))
    p["dww"] = np.ascontiguousarray(np.asarray(inp["od_dw_w"][0], f).reshape(31, 8, 128).transpose(2, 1, 0))
    p["dwb"] = np.ascontiguousarray(np.asarray(inp["od_dw_b"][0], f).reshape(8, 128).T)
    p["cng"] = np.ascontiguousarray(np.asarray(inp["od_conv_norm_g"][0], f).reshape(8, 128).T)
    p["cnb"] = np.ascontiguousarray(np.asarray(inp["od_conv_norm_b"][0], f).reshape(8, 128).T)
    p["dqg"] = np.ascontiguousarray(np.asarray(inp["od_dil_q_norm"][0], f).reshape(128, 1))
    p["dkg"] = np.ascontiguousarray(np.asarray(inp["od_dil_k_norm"][0], f).reshape(128, 1))
    return p


PARAM_SPECS = {
    "wl0": [61, 128, 16, 128], "wl1": [64, 128, 16, 128], "wo0": [4, 128, 16, 512], "wo1": [4, 128, 12, 512],
    "g0": [128, 16], "g1": [128, 16], "retg": [128, 8], "nqg": [128, 1], "nkg": [128, 3],
    "posk": [128, 32], "posv": [128, 32], "w1k": [128, 32, 128], "w1v": [128, 32, 128],
    "w2k": [128, 128], "w2v": [128, 128], "dww": [128, 8, 31], "dwb": [128, 8], "cng": [128, 8],
    "cnb": [128, 8], "dqg": [128, 1], "dkg": [128, 1],
}


import os
ARW = int(os.environ.get("ARW", "88064"))
NW = 4
XNT_END = 16 * S


class Arena:
    def __init__(self, ap):
        self.ap = ap
        self.off = 0

    def reset(self, off=0):
        if os.environ.get("ARDBG") and self.off:
            print("arena phase end off", self.off)
        self.off = off

    def take(self, n, dt):
        nb = n * 2 if dt == F32 else n
        nb += nb % 2
        assert self.off + nb <= ARW, ("arena overflow", self.off, nb)
        a = self.ap[:, self.off:self.off + nb]
        self.off += nb
        if dt == F32:
            a = a.bitcast(F32)
        return a[:, 0:n]


def build(nseq=2, layers=(0, 1), dbg=False, phases=None):
    nc = bass.Bass("TRN2", target_bir_lowering=False)
    NT = nseq * S
    dr = {}
    x_d = nc.dram_tensor("x", [NT, D], F32, kind="ExternalInput").ap()
    out_d = nc.dram_tensor("out", [NT, D], F32, kind="ExternalOutput").ap()
    for k, (shp, dt) in CONST_SPECS.items():
        dr[k] = nc.dram_tensor("c_" + k, shp, dt, kind="ExternalInput").ap()
    for k, shp in PARAM_SPECS.items():
        dr[k] = nc.dram_tensor("p_" + k, shp, F32, kind="ExternalInput").ap()
    x1_d = nc.dram_tensor("x1s", [NT, D], F32, kind="Internal").ap()
    mix_d = nc.dram_tensor("mixs", [nseq * 2, 16, 128, S], BF16,
                           kind="ExternalOutput" if dbg else "Internal").ap()

    es = ExitStack()
    with es:
        def sb(name, shape, dt):
            return es.enter_context(nc.sbuf_tensor(name, shape, dt))

        ARt = sb("arena", [128, ARW], BF16)
        WBt = sb("wbr", [128, NW * 2048], BF16)
        IDF = sb("idf", [128, 128], F32)
        IDB = sb("idb", [128, 128], BF16)
        ONESB = sb("onesb", [128, 128], BF16)
        RPERM = sb("rperm", [128, 128], BF16)
        PRM = sb("prm", [128, 128], F32)
        psb = [es.enter_context(nc.psum_tensor("psb%d" % i, [128, 512], F32)) for i in range(8)]
        PJ = Rot([(psb[0], "PJ0"), (psb[1], "PJ1")])
        ST = Rot([(psb[2], "ST0"), (psb[3], "ST1"), (psb[6], "ST2")])
        OA = Rot([(psb[4], "OA0"), (psb[5], "OA1")])
        LA = Rot([(psb[7], "LA0")])
        SKEW = int(os.environ.get("SKEW", "2"))
        ar = Arena(ARt[:, :])
        WB = WBt[:, :].rearrange("p (s k c) -> p s k c", s=NW, k=16)
        P = Prog(nc)

        def mm(out, lhsT, rhs, start, stop, reads, writes):
            P.pe(lambda e: e.matmul(out, lhsT=lhsT, rhs=rhs, start=start, stop=stop), reads, writes)

        def TR(out, in_, reads, writes):
            P.pe(lambda e: e.transpose(out=out, in_=in_, identity=IDF[:, :]), list(reads) + ["IDF"], writes)

        def ACT(out, in_, func, reads, writes, scale=None, bias=None, accum=None):
            kw = {}
            if scale is not None:
                kw["scale"] = scale
            if bias is not None:
                kw["bias"] = bias
            if accum is not None:
                kw["accum_out"] = accum
            P.act(lambda e: e.activation(out=out, in_=in_, func=func, **kw), reads, writes)

        def TT(out, in0, in1, op, reads, writes):
            P.dve(lambda e: e.tensor_tensor(out=out, in0=in0, in1=in1, op=op), reads, writes)

        def PTT(out, in0, in1, op, reads, writes):
            P.pool(lambda e: e.tensor_tensor(out=out, in0=in0, in1=in1, op=op), reads, writes)

        def TS(out, in0, s1, s2, op0, op1, reads, writes):
            if op1 is None:
                P.dve(lambda e: e.tensor_scalar(out=out, in0=in0, scalar1=s1, scalar2=None, op0=op0), reads, writes)
            else:
                P.dve(lambda e: e.tensor_scalar(out=out, in0=in0, scalar1=s1, scalar2=s2, op0=op0, op1=op1), reads, writes)

        def STT(out, in0, scalar, in1, op0, op1, reads, writes):
            P.dve(lambda e: e.scalar_tensor_tensor(out=out, in0=in0, scalar=scalar, in1=in1, op0=op0, op1=op1), reads, writes)

        def RECIP(out, in_, reads, writes):
            P.dve(lambda e: e.reciprocal(out=out, in_=in_), reads, writes)

        def DMA(eng, out, in_, reads, writes, is_out=False):
            P.dma(eng, lambda e: e.dma_start(out=out, in_=in_), reads, writes, is_out=is_out)

        MUL, ADD, SUB = ALU.mult, ALU.add, ALU.subtract

        DMA("sp", IDF[:, :], dr["idf"][:, :], [], ["IDF"])
        DMA("sp", ONESB[:, :], dr["onesb"][:, :], [], ["ONESB"])
        DMA("sp", RPERM[:, :], dr["rperm"][:, :], [], ["RPERM"])
        ACT(IDB[:, :], IDF[:, :], AF.Copy, ["IDF"], ["IDB"])
        pcol = {"g0": (0, 16), "g1": (16, 16), "retg": (32, 8), "nqg": (40, 1), "nkg": (41, 3),
                "dwb": (44, 8), "cng": (52, 8), "cnb": (60, 8), "dqg": (68, 1), "dkg": (69, 1)}
        for k, (c0, n) in pcol.items():
            DMA("sp", PRM[:, c0:c0 + n], dr[k][:, :], [], ["PRM"])

        def prm(k, j=0, n=1):
            c0 = pcol[k][0] + j
            return PRM[:, c0:c0 + n]

        wlist = []
        for seq in range(nseq):
            for ly in layers:
                nt = 61 if ly == 0 else 64
                for i in range(nt):
                    wlist.append(dr["wl%d" % ly][i])
        wstate = {"next": 0, "loaded": 0}

        def next_w():
            i = wstate["next"]
            wstate["next"] += 1
            upto = min(i + NW - 2, len(wlist) - 1)
            while wstate["loaded"] <= upto:
                j = wstate["loaded"]
                s = j % NW
                DMA("pool", WB[:, s], wlist[j], [], ["WB%d" % s])
                wstate["loaded"] += 1
            s = i % NW
            return WB[:, s], "WB%d" % s

        XNT = None

        def proj_fm(w, tb, M=128):
            wa, wt = w
            ps, pt = PJ.next()
            for kc in range(16):
                mm(ps[0:M, :], wa[:, kc, 0:M], XNT[:, kc, tb * 512:(tb + 1) * 512], kc == 0, kc == 15,
                   [wt, "XNT"], [pt])
            return ps, pt

        def proj_tm(w, tok_aps):
            wa, wt = w
            ps, pt = PJ.next()
            for j, tap in enumerate(tok_aps):
                for kc in range(16):
                    mm(ps[:, j * 128:(j + 1) * 128], tap(kc), wa[:, kc, :], kc == 0, kc == 15, [wt, "XNT"], [pt])
            return ps, pt

        def nat_tok(tt):
            return lambda kc: XNT[:, kc, tt * 128:(tt + 1) * 128]

        def phaseA(xin_d, seq, gname):
            ar.reset(XNT_END)
            XT = [ar.take(D, F32) for _ in range(2)]
            XS = ar.take(D, F32)
            stt = ar.take(4, F32)
            for tt in range(16):
                xt = XT[tt % 2]
                xk = "XT%d" % (tt % 2)
                r0 = seq * S + tt * 128
                DMA("sp", xt, xin_d[r0:r0 + 128, :], [], [xk])
                ACT(XS, xt, AF.Square, [xk], ["XS", "st"], accum=stt[:, 0:1])
                TS(stt[:, 1:2], stt[:, 0:1], 1.0 / D, EPS, MUL, ADD, ["st"], ["st"])
                ACT(stt[:, 1:2], stt[:, 1:2], AF.Ln, ["st"], ["st"])
                ACT(stt[:, 2:3], stt[:, 1:2], AF.Exp, ["st"], ["st"], scale=-0.5)
                ACT(XS, xt, AF.Copy, [xk, "st"], ["XS"], scale=stt[:, 2:3])
                for q in range(4):
                    ps, pt = PJ.next()
                    for j in range(4):
                        kc = 4 * q + j
                        TR(ps[:, j * 128:(j + 1) * 128], XS[:, kc * 128:(kc + 1) * 128], ["XS"], [pt])
                    TT(XNT[:, 4 * q:4 * q + 4, tt * 128:(tt + 1) * 128],
                       ps[:, :].rearrange("p (a b) -> p a b", a=4),
                       prm(gname, 4 * q, 4).unsqueeze(2).to_broadcast([128, 4, 128]), MUL,
                       [pt, "PRM"], ["XNT"])

        def norm_fm(ps, pt, N, gain, out, outtok, SQ, RS):
            ACT(SQ[:, 0:N], ps[:, 0:N], AF.Square, [pt], ["SQ"])
            la, lat = LA.next()
            mm(la[:, 0:N], ONESB[:, :], SQ[:, 0:N], True, True, ["SQ", "ONESB"], [lat])
            ACT(RS[:, 0:N], la[:, 0:N], AF.Ln, [lat], ["RS"], scale=1.0 / 128, bias=EPS)
            ACT(RS[:, 0:N], RS[:, 0:N], AF.Exp, ["RS"], ["RS"], scale=-0.5)
            STT(out, ps[:, 0:N], gain, RS[:, 0:N], MUL, MUL, [pt, "RS", "PRM"], [outtok])

        def retention(seq, mixd):
            ar.reset(XNT_END)
            QT = ar.take(S, BF16); KT = ar.take(S, BF16); ZS = ar.take(S, BF16)
            VT = ar.take(S, BF16).rearrange("p (t e) -> p t e", t=16)
            RCC = ar.take(S, F32); RSS = ar.take(S, F32)
            RT = [ar.take(5 * 512, F32).rearrange("p (v n) -> p v n", v=5) for _ in range(2)]
            QRAW = ar.take(512, BF16)
            TA = ar.take(512, F32); TB = ar.take(512, F32)
            PTs = [ar.take(512, BF16) for _ in range(4)]
            PTR = Rot([(PTs[i], "PT%d" % i) for i in range(4)])
            SQ = ar.take(512, BF16); RS = ar.take(512, F32)
            MH = [ar.take(S, BF16) for _ in range(2)]
            DMA("sp", RCC, dr["rcc"][:, :], [], ["RCC"])
            DMA("sp", RSS, dr["rss"][:, :], [], ["RSS"])
            for h in range(int(os.environ.get('RET_HEADS', '8'))):
                gam = 1.0 - 2.0 ** (-5.0 - h)
                rt = RT[h % 2]; rtk = "RT%d" % (h % 2)
                DMA("sp", rt, dr["rtab"][h], [], [rtk])
                RSTOP = float(os.environ.get("RET_STOP", "99"))
                if RSTOP <= 1:
                    continue
                for (dst, dtok) in ((QT, "QT"), (KT, "KT")):
                    w = next_w()
                    for tb in range(4):
                        blk = slice(tb * 512, (tb + 1) * 512)
                        if RSTOP <= 1.2:
                            continue
                        ps, pt = proj_fm(w, tb)
                        ACT(QRAW, ps[:, :], AF.Copy, [pt], ["QRAW"])
                        if RSTOP <= 1.5:
                            continue
                        ps2, pt2 = ST.next()
                        mm(ps2[:, :], RPERM[:, :], QRAW, True, True, ["QRAW", "RPERM"], [pt2])
                        if RSTOP <= 1.7:
                            continue
                        if os.environ.get("VARA"):
                            TT(TA, QRAW, RCC[:, blk], MUL, ["QRAW", "RCC"], ["TA"])
                        else:
                            TT(TA, ps[:, :], RCC[:, blk], MUL, [pt, "RCC"], ["TA"])
                        if RSTOP <= 1.8:
                            continue
                        TT(TB, ps2[:, :], RSS[:, blk], MUL, [pt2, "RSS"], ["TB"])
                        if RSTOP <= 1.9:
                            continue
                        TT(dst[:, blk], TA, TB, ADD, ["TA", "TB"], [dtok])
                if RSTOP <= 2:
                    continue
                w = next_w()
                for t4 in range(4):
                    ps, pt = proj_tm(w, [nat_tok(4 * t4 + j) for j in range(4)])
                    ACT(VT[:, 4 * t4:4 * t4 + 4, :], ps[:, :].rearrange("p (a b) -> p a b", a=4), AF.Copy, [pt], ["VT"])
                if RSTOP <= 3:
                    continue
                w = next_w()
                for tb in range(4):
                    ps, pt = proj_fm(w, tb)
                    ACT(ZS[:, tb * 512:(tb + 1) * 512], ps[:, :], AF.Silu, [pt], ["ZS"])
                if RSTOP <= 4:
                    continue
                mh = MH[h % 2]; mhk = "MH%d" % (h % 2)
                for tb in range(4):
                    blk = slice(tb * 512, (tb + 1) * 512)
                    nk = 4 * tb + 4
                    oa, oat = OA.next()

                    def pv(pr, nk=nk, oa=oa, oat=oat):
                        kt, pa, pk = pr
                        mm(oa[:, :], VT[:, kt, :], pa, kt == 0, kt == nk - 1, ["VT", pk], [oat])

                    pend = []
                    for kt in range(nk):
                        st, sk = ST.next()
                        mm(st[:, :], KT[:, kt * 128:(kt + 1) * 128], QT[:, blk], True, True, ["KT", "QT"], [sk])
                        if len(pend) >= SKEW:
                            pv(pend.pop(0))
                        r = kt - 4 * tb
                        pa, pk = PTR.next()
                        if r < 0:
                            STT(pa, st[:, :], float(gam ** (-128 * r) * SCALE), rt[:, 0, :], MUL, MUL, [sk, rtk], [pk])
                        else:
                            STT(pa, st[:, :], float(SCALE), rt[:, 1 + r, :], MUL, MUL, [sk, rtk], [pk])
                        pend.append((kt, pa, pk))
                    for pr in pend:
                        pv(pr)
                    ACT(SQ, oa[:, :], AF.Square, [oat], ["SQ"])
                    la, lat = LA.next()
                    mm(la[:, :], ONESB[:, :], SQ, True, True, ["SQ", "ONESB"], [lat])
                    ACT(RS, la[:, :], AF.Ln, [lat], ["RS"], scale=1.0 / 128, bias=EPS)
                    ACT(RS, RS, AF.Exp, ["RS"], ["RS"], scale=-0.5)
                    STT(TA, oa[:, :], prm("retg", h), RS, MUL, MUL, [oat, "RS", "PRM"], ["TA"])
                    TT(mh[:, blk], TA, ZS[:, blk], MUL, ["TA", "ZS"], [mhk])
                DMA("sp", mixd[h], mh, [mhk], ["mixd"])

        def nsa(seq, mixd):
            ar.reset(XNT_END)
            KZ = ar.take(S, BF16)
            VB = ar.take(S, BF16)
            KST = ar.take(S, BF16); KWT = ar.take(S, BF16)
            VS = ar.take(S, BF16).rearrange("p (t e) -> p t e", t=16)
            VW = ar.take(S, BF16).rearrange("p (t e) -> p t e", t=16)
            QN = ar.take(4 * S, BF16).rearrange("p (r n) -> p r n", r=4)
            SG = ar.take(S, BF16)
            W1K = ar.take(4096, BF16).rearrange("p (l e) -> p l e", l=32)
            W1V = ar.take(4096, BF16).rearrange("p (l e) -> p l e", l=32)
            W2K = ar.take(128, BF16); W2V = ar.take(128, BF16)
            POSK = ar.take(32, BF16); POSV = ar.take(32, BF16)
            GG = ar.take(128, BF16); KCN = ar.take(128, BF16); VC = ar.take(128, BF16)
            CM = ar.take(2048, BF16).rearrange("p (r n) -> p r n", r=4)
            WM = ar.take(2048, BF16).rearrange("p (r n) -> p r n", r=4)
            VALID = ar.take(S, BF16)
            OVL = ar.take(32, BF16)
            FTAB = ar.take(512, F32).rearrange("p (t j) -> p t j", t=16)
            EXPD = ar.take(2048, BF16).rearrange("p (t m) -> p t m", t=16)
            SEL = ar.take(24 * 128, BF16).rearrange("p (c m) -> p c m", c=24)
            PTs = [ar.take(512, BF16) for _ in range(4)]
            PTR = Rot([(PTs[i], "PT%d" % i) for i in range(4)])
            PN = ar.take(512, BF16)
            SQ = ar.take(512, BF16); RS = ar.take(512, F32)
            RL = ar.take(512, F32); CF = ar.take(512, F32); ACC = ar.take(512, F32); TMP = ar.take(512, F32)
            U = ar.take(128, F32); U2 = ar.take(128, F32); B1 = ar.take(2, F32)
            IA = ar.take(32, F32); IB = ar.take(32, F32); M8 = ar.take(8, F32); M8b = ar.take(8, F32)
            SELM = ar.take(32, F32)
            MH = [ar.take(S, BF16)] * 2
            for (dst, key, tok) in ((CM, "cm", "CM"), (WM, "wm", "WM"), (EXPD, "expd", "EXPD"), (SEL, "sel", "SEL"),
                                    (FTAB, "ftab", "FTAB")):
                src = dr[key]
                if key in ("expd", "sel"):
                    DMA("sp", dst[0:32], src, [], [tok])
                else:
                    DMA("sp", dst, src, [], [tok])
            DMA("sp", VALID, dr["valid"][:, :], [], ["VALID"])
            DMA("sp", OVL, dr["ovl"][:, :], [], ["OVL"])
            DMA("pool", W1K, dr["w1k"], [], ["W1K"])
            DMA("pool", W1V, dr["w1v"], [], ["W1V"])
            DMA("pool", W2K, dr["w2k"][:, :], [], ["W2K"])
            DMA("pool", W2V, dr["w2v"][:, :], [], ["W2V"])
            DMA("pool", POSK, dr["posk"][:, :], [], ["POSK"])
            DMA("pool", POSV, dr["posv"][:, :], [], ["POSV"])

            def compress(raw, rawtok, W1, w1t, POS, post, W2, w2t, is_k):
                pb, pbt = PJ.next()
                for l in range(32):
                    mm(pb[:, 0:1], W1[:, l, :], POS[:, l:l + 1], l == 0, l == 31, [w1t, post], [pbt])
                ACT(B1[:, 0:1], pb[:, 0:1], AF.Copy, [pbt], ["B1"])
                ph, pht = PJ.next()
                rv = raw.rearrange("p (c s) -> p c s", s=16)
                for l in range(32):
                    rhs = rv[:, 0:127, l] if l < 16 else rv[:, 1:128, l - 16]
                    mm(ph[:, 0:127], W1[:, l, :], rhs, l == 0, l == 31, [w1t, rawtok], [pht])
                u, u2 = U[:, 0:127], U2[:, 0:127]
                TS(u, ph[:, 0:127], B1[:, 0:1], None, ADD, None, [pht, "B1"], ["U"])
                TT(u2, u, u, MUL, ["U"], ["U2"])
                TS(u2, u2, 0.044715, 1.0, MUL, ADD, ["U2"], ["U2"])
                TT(u2, u2, u, MUL, ["U2", "U"], ["U2"])
                ACT(u2, u2, AF.Sigmoid, ["U2"], ["U2"], scale=1.5957691216057308)
                TT(GG[:, 0:127], u, u2, MUL, ["U", "U2"], ["GG"])
                if is_k:
                    pk_, pkt = PJ.next()
                    mm(pk_[:, 0:127], W2, GG[:, 0:127], True, True, [w2t, "GG"], [pkt])
                    norm_fm(pk_, pkt, 127, prm("nkg", 0), KCN[:, 0:127], "KCN", SQ, RS)
                else:
                    pv_, pvt = PJ.next()
                    mm(pv_[0:127, 0:128], GG[:, 0:127], W2, True, True, [w2t, "GG"], [pvt])
                    ACT(VC[0:127, :], pv_[0:127, 0:128], AF.Copy, [pvt], ["VC"])

            def attn(qap, tiles, oa, oat, la, lat):
                n = len(tiles)

                def pv(pr):
                    i, t, pa, pk = pr
                    nk = t["nk"]
                    mm(oa[:, :], t["v"][0], pa[0:nk, :], i == 0, i == n - 1, [t["v"][1], pk], [oat])
                    mm(la[:, :], ONESB[0:nk, :], pa[0:nk, :], i == 0, i == n - 1, ["ONESB", pk], [lat])

                pend = []
                for i, t in enumerate(tiles):
                    st, sk = ST.next()
                    nk = t["nk"]
                    b = t.get("bias")
                    mm(st[0:nk, :], t["k"][0], qap, True, b is None, [t["k"][1], "QN"], [sk])
                    if b is not None:
                        mm(st[0:nk, :], b[0], b[1], False, True, ["EXPD", "VB"], [sk])
                    if len(pend) >= SKEW:
                        pv(pend.pop(0))
                    pa, pk = PTR.next()
                    ACT(pa[0:nk, :], st[0:nk, :], AF.Exp, [sk], [pk], scale=float(SCALE))
                    m = t.get("mask")
                    if m is not None:
                        if t.get("pool") and os.environ.get("POOLMASK", "1") == "1":
                            PTT(pa[0:nk, :], pa[0:nk, :], m[0], MUL, [pk, m[1]], [pk])
                        else:
                            TT(pa[0:nk, :], pa[0:nk, :], m[0], MUL, [pk, m[1]], [pk])
                    pend.append((i, t, pa, pk))
                for pr in pend:
                    pv(pr)

            def branch_fin(oa, oat, la, lat, gcol, blk, first):
                pg, pgt = PJ.next()
                mm(pg[:, :], SEL[0:32, gcol, :], SG[0:32, blk], True, True, ["SEL", "SG"], [pgt])
                if first:
                    ACT(RL, la[:, :], AF.Ln, [lat], ["RL"], bias=1e-18)
                else:
                    ACT(RL, la[:, :], AF.Ln, [lat], ["RL"])
                ACT(RL, RL, AF.Exp, ["RL"], ["RL"], scale=-1.0)
                TT(CF, RL, pg[:, :], MUL, ["RL", pgt], ["CF"])
                if first:
                    TT(ACC, oa[:, :], CF, MUL, [oat, "CF"], ["ACC"])
                else:
                    TT(TMP, oa[:, :], CF, MUL, [oat, "CF"], ["TMP"])
                    TT(ACC, ACC, TMP, ADD, ["ACC", "TMP"], ["ACC"])

            for g in range(2):
                w = next_w()
                for tb in range(4):
                    ps, pt = proj_fm(w, tb)
                    ACT(KZ[:, tb * 512:(tb + 1) * 512], ps[:, :], AF.Copy, [pt], ["KZ"])
                w = next_w()
                for tb in range(4):
                    ps, pt = proj_fm(w, tb)
                    ACT(VB[:, tb * 512:(tb + 1) * 512], ps[:, :], AF.Copy, [pt], ["VB"])
                w = next_w()
                for tb in range(4):
                    ps, pt = proj_fm(w, tb)
                    norm_fm(ps, pt, 512, prm("nkg", 1), KST[:, tb * 512:(tb + 1) * 512], "KST", SQ, RS)
                w = next_w()
                for t4 in range(4):
                    ps, pt = proj_tm(w, [nat_tok(4 * t4 + j) for j in range(4)])
                    ACT(VS[:, 4 * t4:4 * t4 + 4, :], ps[:, :].rearrange("p (a b) -> p a b", a=4), AF.Copy, [pt], ["VS"])
                w = next_w()
                for tb in range(4):
                    ps, pt = proj_fm(w, tb)
                    norm_fm(ps, pt, 512, prm("nkg", 2), KWT[:, tb * 512:(tb + 1) * 512], "KWT", SQ, RS)
                w = next_w()
                for t4 in range(4):
                    ps, pt = proj_tm(w, [nat_tok(4 * t4 + j) for j in range(4)])
                    ACT(VW[:, 4 * t4:4 * t4 + 4, :], ps[:, :].rearrange("p (a b) -> p a b", a=4), AF.Copy, [pt], ["VW"])
                if g == 0:
                    w = next_w()
                    for tb in range(4):
                        ps, pt = proj_fm(w, tb, M=32)
                        ACT(SG[0:32, tb * 512:(tb + 1) * 512], ps[0:32, :], AF.Sigmoid, [pt], ["SG"])
                compress(KZ, "KZ", W1K, "W1K", POSK, "POSK", W2K, "W2K", True)
                compress(VB, "VB", W1V, "W1V", POSV, "POSV", W2V, "W2V", False)
                for r in range(4):
                    w = next_w()
                    for tb in range(4):
                        ps, pt = proj_fm(w, tb)
                        norm_fm(ps, pt, 512, prm("nqg", 0), QN[:, r, tb * 512:(tb + 1) * 512], "QN", SQ, RS)
                for tb in (2, 3):
                    blk = slice(tb * 512, (tb + 1) * 512)
                    pim, pimt = PJ.next()
                    pns = []
                    for r in range(4):
                        st, sk = ST.next()
                        mm(st[0:127, :], KCN[:, 0:127], QN[:, r, blk], True, True, ["KCN", "QN"], [sk])
                        pa, pk = PTR.next()
                        ACT(pa[0:127, :], st[0:127, :], AF.Exp, [sk], [pk], scale=float(SCALE))
                        TT(pa[0:127, :], pa[0:127, :], VALID[0:127, blk], MUL, [pk, "VALID"], [pk])
                        la, lat = LA.next()
                        mm(la[:, :], ONESB[0:127, :], pa[0:127, :], True, True, ["ONESB", pk], [lat])
                        ACT(RL, la[:, :], AF.Ln, [lat], ["RL"], bias=1e-18)
                        ACT(RL, RL, AF.Exp, ["RL"], ["RL"], scale=-1.0)
                        TT(pa[0:127, :], pa[0:127, :], RL[0:127, :], MUL, [pk, "RL"], [pk])
                        pns.append((pa, pk))
                    for j in range(4):
                        for r in range(4):
                            pa, pk = pns[r]
                            mm(pim[:, j * 32:(j + 1) * 32], pa[0:127, j * 128:(j + 1) * 128], OVL[0:127, :],
                               r == 0, r == 3, [pk, "OVL"], [pimt])
                    for j in range(4):
                        tt = 4 * tb + j
                        TT(IA, pim[:, j * 32:(j + 1) * 32], FTAB[:, tt, :], ADD, [pimt, "FTAB"], ["IA"])
                        P.dve(lambda e: e.max(out=M8, in_=IA), ["IA"], ["M8"])
                        P.dve(lambda e: e.match_replace(out=IB, in_to_replace=M8, in_values=IA, imm_value=-3.0e38),
                              ["IA", "M8"], ["IB"])
                        P.dve(lambda e: e.max(out=M8b, in_=IB), ["IB"], ["M8b"])
                        TS(SELM, IA, M8b[:, 7:8], 1.0, ALU.is_ge, SUB, ["IA", "M8b"], ["SELM"])
                        ptp, ptpt = PJ.next()
                        TR(ptp[0:32, 0:128], SELM, ["SELM"], [ptpt])
                        ACT(VB[0:32, tt * 128:(tt + 1) * 128], ptp[0:32, 0:128], AF.Copy, [ptpt], ["VB"])
                for r in range(4):
                    h = 4 * g + r
                    w = next_w()
                    for tb in range(4):
                        ps, pt = proj_fm(w, tb)
                        ACT(KZ[:, tb * 512:(tb + 1) * 512], ps[:, :], AF.Silu, [pt], ["KZ"])
                    mh = MH[0]; mhk = "MH0"
                    for tb in range(4):
                        blk = slice(tb * 512, (tb + 1) * 512)
                        qap = QN[:, r, blk]
                        oa, oat = OA.next(); la, lat = LA.next()
                        attn(qap, [dict(k=(KCN[:, 0:127], "KCN"), nk=127, v=(VC[0:127, :], "VC"),
                                        mask=(VALID[0:127, blk], "VALID"))], oa, oat, la, lat)
                        branch_fin(oa, oat, la, lat, 0 * 8 + h, blk, True)
                        tiles = []
                        for kt in range(4 * tb + 4):
                            t = dict(k=(KST[:, kt * 128:(kt + 1) * 128], "KST"), nk=128, v=(VS[:, kt, :], "VS"))
                            if tb >= 2:
                                t["bias"] = (EXPD[0:32, kt, :], VB[0:32, blk])
                            rr = kt - 4 * tb
                            if rr >= 0:
                                t["mask"] = (CM[:, rr, :], "CM")
                            tiles.append(t)
                        oa, oat = OA.next(); la, lat = LA.next()
                        attn(qap, tiles, oa, oat, la, lat)
                        branch_fin(oa, oat, la, lat, 1 * 8 + h, blk, False)
                        tiles = []
                        for kt in range(max(0, 4 * tb - 4), 4 * tb + 4):
                            rr = kt - 4 * tb
                            m = (CM[:, rr, :], "CM") if rr >= 0 else (WM[:, rr + 4, :], "WM")
                            tiles.append(dict(k=(KWT[:, kt * 128:(kt + 1) * 128], "KWT"), nk=128, v=(VW[:, kt, :], "VW"), mask=m,
                                              pool=(kt % 2 == 0)))
                        oa, oat = OA.next(); la, lat = LA.next()
                        attn(qap, tiles, oa, oat, la, lat)
                        branch_fin(oa, oat, la, lat, 2 * 8 + h, blk, False)
                        TT(mh[:, blk], ACC, KZ[:, blk], MUL, ["ACC", "KZ"], [mhk])
                    DMA("sp", mixd[8 + h], mh, [mhk], ["mixd"])

        def conv_branch(seq, mixd):
            ar.reset(XNT_END)
            A = ar.take(8 * S, F32).rearrange("p (j n) -> p j n", j=8)
            A0 = ar.take(S + 32, BF16)
            DG = ar.take(31 * 128, BF16).rearrange("p (k m) -> p k m", k=31)
            DWW = ar.take(8 * 31, F32).rearrange("p (j k) -> p j k", j=8)
            SGT = ar.take(512, F32)
            MU = ar.take(S, F32); RSTD = ar.take(S, F32)
            TBF = [ar.take(512, BF16) for _ in range(2)]
            TSQ = [ar.take(512, BF16) for _ in range(2)]
            TV = SGT; T1 = ar.take(512, F32); T2 = ar.take(512, F32)
            ZSb = ar.take(512, BF16)
            MH = [ar.take(S, BF16)] * 2
            DMA("sp", DWW, dr["dww"], [], ["DWW"])
            P.dve(lambda e: e.memset(A0[:, 0:32], 0.0), [], ["A0"])
            for j in range(8):
                wu = next_w(); wg = next_w()
                for k in range(31):
                    TS(DG[:, k, :], IDB[:, :], DWW[:, j, k:k + 1], None, MUL, None, ["IDB", "DWW"], ["DG"])
                for tb in range(4):
                    pu, put = proj_fm(wu, tb)
                    pg, pgt = proj_fm(wg, tb)
                    ACT(SGT, pg[:, :], AF.Sigmoid, [pgt], ["SGT"])
                    TT(A0[:, 32 + tb * 512:32 + (tb + 1) * 512], pu[:, :], SGT, MUL, [put, "SGT"], ["A0"])
                for tb in range(4):
                    pc, pct = ST.next()
                    for k in range(31):
                        mm(pc[:, :], DG[:, k, :], A0[:, 2 + k + tb * 512:2 + k + (tb + 1) * 512], k == 0, k == 30,
                           ["DG", "A0"], [pct])
                    ACT(A[:, j, tb * 512:(tb + 1) * 512], pc[:, :], AF.Identity, [pct, "PRM"], ["A"], bias=prm("dwb", j))
            for tb in range(4):
                blk = slice(tb * 512, (tb + 1) * 512)
                s1, s1t = LA.next(); s2, s2t = OA.next()
                for j in range(8):
                    ACT(TBF[j % 2], A[:, j, blk], AF.Copy, ["A"], ["TBF%d" % (j % 2)])
                    ACT(TSQ[j % 2], A[:, j, blk], AF.Square, ["A"], ["TSQ%d" % (j % 2)])
                    mm(s1[:, :], ONESB[:, :], TBF[j % 2], j == 0, j == 7, ["ONESB", "TBF%d" % (j % 2)], [s1t])
                    mm(s2[:, :], ONESB[:, :], TSQ[j % 2], j == 0, j == 7, ["ONESB", "TSQ%d" % (j % 2)], [s2t])
                TS(MU[:, blk], s1[:, :], 1.0 / 1024, None, MUL, None, [s1t], ["MU"])
                TT(TV, MU[:, blk], MU[:, blk], MUL, ["MU"], ["SGT"])
                STT(TV, s2[:, :], 1.0 / 1024, TV, MUL, SUB, [s2t, "SGT"], ["SGT"])
                ACT(TV, TV, AF.Ln, ["SGT"], ["SGT"], bias=EPS)
                ACT(RSTD[:, blk], TV, AF.Exp, ["SGT"], ["RSTD"], scale=-0.5)
            for j in range(8):
                wz = next_w()
                mh = MH[0]; mhk = "MH0"
                for tb in range(4):
                    blk = slice(tb * 512, (tb + 1) * 512)
                    pz, pzt = proj_fm(wz, tb)
                    ACT(ZSb, pz[:, :], AF.Silu, [pzt], ["ZSb"])
                    TT(T1, A[:, j, blk], MU[:, blk], SUB, ["A", "MU"], ["T1"])
                    TT(T1, T1, RSTD[:, blk], MUL, ["T1", "RSTD"], ["T1"])
                    ACT(T2, T1, AF.Silu, ["T1", "PRM"], ["T2"], scale=prm("cng", j), bias=prm("cnb", j))
                    TT(mh[:, blk], T2, ZSb, MUL, ["T2", "ZSb"], [mhk])
                DMA("sp", mixd[j], mh, [mhk], ["mixd"])

        def dilated(seq, mixd):
            ar.reset(XNT_END)
            QN = ar.take(S, BF16); KN = ar.take(S, BF16)
            VD = ar.take(S, BF16).rearrange("p (t e) -> p t e", t=16)
            OACC = ar.take(S, F32); LACC = ar.take(S, F32)
            DM = ar.take(1024, BF16).rearrange("p (v n) -> p v n", v=2)
            PTs = [ar.take(512, BF16) for _ in range(3)]
            PTR = Rot([(PTs[i], "PT%d" % i) for i in range(3)])
            SQ = ar.take(512, BF16); RS = ar.take(512, F32)
            RL = ar.take(512, F32); T1 = ar.take(512, F32); ZSb = ar.take(512, BF16)
            MH = [ar.take(S, BF16) for _ in range(2)]
            DMA("sp", DM, dr["dm"], [], ["DM"])
            for hh in range(4):
                for gi, dil in enumerate((1, 4, 16)):
                    L = S // dil
                    nqt = L // 128

                    def sub(buf, c, qt, dil=dil):
                        return buf.rearrange("p (a d) -> p a d", d=dil)[:, 128 * qt:128 * qt + 128, c]

                    w = next_w()
                    for tb in range(4):
                        ps, pt = proj_fm(w, tb)
                        norm_fm(ps, pt, 512, prm("dqg", 0), QN[:, tb * 512:(tb + 1) * 512], "QN", SQ, RS)
                    w = next_w()
                    for tb in range(4):
                        ps, pt = proj_fm(w, tb)
                        norm_fm(ps, pt, 512, prm("dkg", 0), KN[:, tb * 512:(tb + 1) * 512], "KN", SQ, RS)
                    w = next_w()
                    QL = [(c, qt) for c in range(dil) for qt in range(nqt)]
                    for t4 in range(4):
                        taps = []
                        for j in range(4):
                            c, qt = QL[4 * t4 + j]
                            taps.append(lambda kc, c=c, qt=qt: sub(XNT[:, kc, :], c, qt))
                        ps, pt = proj_tm(w, taps)
                        ACT(VD[:, 4 * t4:4 * t4 + 4, :], ps[:, :].rearrange("p (a b) -> p a b", a=4), AF.Copy, [pt], ["VD"])
                    for q4 in range(4):
                        oa, oat = OA.next(); la, lat = LA.next()
                        for half in range(2):
                            st, sk = ST.next()
                            info = []
                            for u in range(2):
                                ti = 4 * q4 + 2 * half + u
                                c, qt = QL[ti]
                                tp = ti - 1 if qt > 0 else ti
                                cp, qp = QL[tp]
                                qap = sub(QN, c, qt)
                                mm(st[:, u * 256:u * 256 + 128], sub(KN, cp, qp), qap, True, True, ["KN", "QN"], [sk])
                                mm(st[:, u * 256 + 128:u * 256 + 256], sub(KN, c, qt), qap, True, True, ["KN", "QN"], [sk])
                                info.append((ti, tp, qt))
                            pa, pk = PTR.next()
                            ACT(pa, st[:, :], AF.Exp, [sk], [pk], scale=float(SCALE))
                            for u in range(2):
                                ti, tp, qt = info[u]
                                v = 0 if qt > 0 else 1
                                TT(pa[:, u * 256:u * 256 + 256], pa[:, u * 256:u * 256 + 256], DM[:, v, 0:256] if v == 0 else DM[:, 1, 0:256],
                                   MUL, [pk, "DM"], [pk])
                            for u in range(2):
                                ti, tp, qt = info[u]
                                col = (2 * half + u) * 128
                                mm(oa[:, col:col + 128], VD[:, tp, :], pa[:, u * 256:u * 256 + 128], True, False, ["VD", pk], [oat])
                                mm(oa[:, col:col + 128], VD[:, ti, :], pa[:, u * 256 + 128:u * 256 + 256], False, True, ["VD", pk], [oat])
                                mm(la[:, col:col + 128], ONESB[:, :], pa[:, u * 256:u * 256 + 128], True, False, ["ONESB", pk], [lat])
                                mm(la[:, col:col + 128], ONESB[:, :], pa[:, u * 256 + 128:u * 256 + 256], False, True, ["ONESB", pk], [lat])
                        if nqt >= 4:
                            c = (4 * q4) // nqt
                            qt0 = (4 * q4) % nqt

                            def dst(buf, c=c, qt0=qt0, dil=dil):
                                return buf.rearrange("p (a d) -> p a d", d=dil)[:, 128 * qt0:128 * qt0 + 512, c]

                            srcv = lambda p_: p_[:, :]
                        else:
                            def dst(buf, q4=q4):
                                return buf.rearrange("p (a d) -> p d a", d=16)[:, 4 * q4:4 * q4 + 4, :]

                            srcv = lambda p_: p_[:, :].rearrange("p (u i) -> p u i", u=4)
                        if gi == 0:
                            ACT(dst(OACC), srcv(oa), AF.Copy, [oat], ["OACC"])
                            ACT(dst(LACC), srcv(la), AF.Copy, [lat], ["LACC"])
                        else:
                            TT(dst(OACC), srcv(oa), dst(OACC), ADD, [oat, "OACC"], ["OACC"])
                            TT(dst(LACC), srcv(la), dst(LACC), ADD, [lat, "LACC"], ["LACC"])
                w = next_w()
                mh = MH[hh % 2]; mhk = "MH%d" % (hh % 2)
                for tb in range(4):
                    blk = slice(tb * 512, (tb + 1) * 512)
                    pz, pzt = proj_fm(w, tb)
                    ACT(ZSb, pz[:, :], AF.Silu, [pzt], ["ZSb"])
                    ACT(RL, LACC[:, blk], AF.Ln, ["LACC"], ["RL"])
                    ACT(RL, RL, AF.Exp, ["RL"], ["RL"], scale=-1.0)
                    TT(T1, OACC[:, blk], RL, MUL, ["OACC", "RL"], ["T1"])
                    TT(mh[:, blk], T1, ZSb, MUL, ["T1", "ZSb"], [mhk])
                DMA("sp", mixd[8 + hh], mh, [mhk], ["mixd"])

        def phaseC(seq, xin_d, xout_d, mixd, wo, nmc, is_out):
            ar.reset(0)
            WO = ar.take(4 * nmc * 512, BF16).rearrange("p (b c n) -> p b c n", b=4, c=nmc)
            XR = [ar.take(D, F32) for _ in range(2)]
            Y = [ar.take(D, F32) for _ in range(2)]
            MT = [ar.take(nmc * 128, BF16).rearrange("p (c t) -> p c t", c=nmc) for _ in range(2)]
            for nb in range(4):
                DMA("pool", WO[:, nb], wo[nb], [], ["WO"])
            for tt in range(16):
                b = tt % 2
                r0 = seq * S + tt * 128
                DMA("sp", MT[b], mixd[0:nmc, :, tt * 128:(tt + 1) * 128].rearrange("c p t -> p c t"), ["mixd"], ["MT%d" % b])
                DMA("sp", XR[b], xin_d[r0:r0 + 128, :], [], ["XR%d" % b])
                for nb in range(4):
                    ps, pt = PJ.next()
                    for mc in range(nmc):
                        mm(ps[:, :], MT[b][:, mc, :], WO[:, nb, mc, :], mc == 0, mc == nmc - 1, ["MT%d" % b, "WO"], [pt])
                    TT(Y[b][:, nb * 512:(nb + 1) * 512], ps[:, :], XR[b][:, nb * 512:(nb + 1) * 512], ADD,
                       [pt, "XR%d" % b], ["Y%d" % b])
                DMA("sp", xout_d[r0:r0 + 128, :], Y[b], ["Y%d" % b], ["xout"], is_out=is_out)

        XNT = ar.take(16 * S, BF16).rearrange("p (k n) -> p k n", k=16)
        nl = len(layers)
        for seq in range(nseq):
            for li, ly in enumerate(layers):
                xin = x_d if li == 0 else x1_d
                xout = out_d if li == nl - 1 else x1_d
                mixd = mix_d[seq * 2 + li]
                on = lambda nm: phases is None or nm in phases
                P.fence()
                if on("A"):
                    phaseA(xin, seq, "g%d" % ly)
                P.fence()
                if ly == 0:
                    if on("ret"):
                        retention(seq, mixd)
                    else:
                        wstate["next"] += 32; wstate["loaded"] = max(wstate["loaded"], wstate["next"])
                    P.fence()
                    if on("nsa"):
                        nsa(seq, mixd)
                else:
                    if on("conv"):
                        conv_branch(seq, mixd)
                    else:
                        wstate["next"] += 24; wstate["loaded"] = max(wstate["loaded"], wstate["next"])
                    P.fence()
                    if on("dil"):
                        dilated(seq, mixd)
                P.fence()
                if on("C"):
                    phaseC(seq, xin, xout, mixd, dr["wo%d" % ly], 16 if ly == 0 else 12, li == nl - 1)
        P.emit()
    return nc


_CACHE = {}


def _prep(inputs, nseq, ncores):
    consts = host_consts()
    params = host_params(inputs)
    x = np.asarray(inputs["x"], np.float32)
    maps = []
    for c in range(ncores):
        m = {"x": np.ascontiguousarray(x[c * nseq:(c + 1) * nseq].reshape(nseq * S, D))}
        for k, v in consts.items():
            m["c_" + k] = v
        for k, v in params.items():
            m["p_" + k] = v
        maps.append(m)
    return maps


def kernel(**inputs):
    ncores, nseq = 8, 2
    if "nc" not in _CACHE:
        _CACHE["nc"] = build(nseq=nseq, layers=(0, 1))
    nc = _CACHE["nc"]
    maps = _prep(inputs, nseq, ncores)
    res = run_bass_kernel_spmd(nc, maps, core_ids=list(range(ncores)))
    outs = [np.asarray(r["out"], np.float32).reshape(nseq, S, D) for r in res.results]
    return np.concatenate(outs, axis=0)
```

```python
import math
import numpy as np
import ml_dtypes
import concourse.bass as bass
import concourse.mybir as mybir
from concourse.bass_utils import run_bass_kernel_spmd
from contextlib import ExitStack

F32 = mybir.dt.float32
BF16 = mybir.dt.bfloat16
AF = mybir.ActivationFunctionType
ALU = mybir.AluOpType

S = 2048
D = 2048
EPS = 1e-6
SCALE = 128 ** -0.5
NEGB = 30000.0


class _Op:
    __slots__ = ("eng", "fn", "deps", "dma", "sig", "val", "sem", "slot")


class Prog:
    ENGS = ("pe", "act", "dve", "pool", "sp")
    NSLOT = 8

    def __init__(self, nc):
        self.nc = nc
        self.ops = []
        self.last_w = {}
        self.readers = {}
        self.out_dmas = []
        self.last_eng = {}
        self.last_slot = {}
        self.dcnt = {e: 0 for e in self.ENGS}
        self.pending = {}

    def fence(self):
        snap = list(self.last_eng.values()) + list(self.last_slot.values())
        for e in self.ENGS:
            self.pending[e] = list(snap)
        self.last_w = {}
        self.readers = {}

    def add(self, eng, fn, reads=(), writes=(), dma=False, is_out=False):
        op = _Op()
        idx = len(self.ops)
        op.eng, op.fn, op.dma = eng, fn, dma
        op.sig = dma
        op.val = 0
        op.sem = None
        op.slot = None
        ex = [t for t in reads if t[:2] in ("PJ", "ST", "OA", "LA")]
        if ex:
            reads = [t for t in reads if t not in ex]
            writes = list(writes) + ex
        deps = set()
        for t in reads:
            w = self.last_w.get(t)
            if w is not None:
                deps.add(w)
        for t in writes:
            w = self.last_w.get(t)
            if w is not None:
                deps.add(w)
            for r in self.readers.get(t, ()):
                deps.add(r)
        pf = self.pending.pop(eng, None)
        if pf:
            deps.update(pf)
        for t in reads:
            self.readers.setdefault(t, []).append(idx)
        for t in writes:
            self.last_w[t] = idx
            self.readers[t] = []
        if dma:
            k = self.dcnt[eng]
            self.dcnt[eng] += 1
            s = k % self.NSLOT
            op.slot = (eng, s, 16 * (k // self.NSLOT + 1))
            prev = self.last_slot.get((eng, s))
            if prev is not None:
                deps.add(prev)
            self.last_slot[(eng, s)] = idx
        keep = []
        for d in deps:
            o = self.ops[d]
            if eng == "pe" and o.eng == "pe" and not o.dma and not dma:
                continue
            keep.append(d)
            o.sig = True
        op.deps = keep
        self.ops.append(op)
        if not dma:
            self.last_eng[eng] = idx
        if is_out:
            self.out_dmas.append(idx)
        return idx

    def pe(self, fn, reads=(), writes=()):
        return self.add("pe", fn, reads, writes)

    def act(self, fn, reads=(), writes=()):
        return self.add("act", fn, reads, writes)

    def dve(self, fn, reads=(), writes=()):
        return self.add("dve", fn, reads, writes)

    def pool(self, fn, reads=(), writes=()):
        return self.add("pool", fn, reads, writes)

    def dma(self, eng, fn, reads=(), writes=(), is_out=False):
        return self.add(eng, fn, reads, writes, dma=True, is_out=is_out)

    def emit(self):
        nc = self.nc
        with ExitStack() as es:
            esem = {e: es.enter_context(nc.semaphore("s_" + e)) for e in self.ENGS if e != "sp"}
            dsem = {e: [es.enter_context(nc.semaphore("d_%s%d" % (e, i))) for i in range(self.NSLOT)]
                    for e in ("sp", "pool", "act")}
            cnt = {e: 0 for e in self.ENGS}
            for op in self.ops:
                if op.dma:
                    e, s, v = op.slot
                    op.sem = dsem[e][s]
                    op.val = v
                elif op.sig:
                    cnt[op.eng] += 1
                    op.sem = esem[op.eng]
                    op.val = cnt[op.eng]
            per_eng = {e: [] for e in self.ENGS}
            for i, op in enumerate(self.ops):
                per_eng[op.eng].append(i)
            ops = self.ops
            out_dmas = self.out_dmas
            block = es.enter_context(nc.Block())

            def run(eng_name, h):
                waited = {}
                for i in per_eng[eng_name]:
                    op = ops[i]
                    need = {}
                    for d in op.deps:
                        o = ops[d]
                        key = id(o.sem)
                        if waited.get(key, 0) >= o.val:
                            continue
                        if key not in need or need[key][1] < o.val:
                            need[key] = (o.sem, o.val)
                    for key, (sem, val) in need.items():
                        h.wait_ge(sem, val)
                        waited[key] = val
                    ins = op.fn(h)
                    if op.sig:
                        ins.then_inc(op.sem, 16 if op.dma else 1)
                if eng_name == "sp":
                    fin = {}
                    for i in out_dmas:
                        o = ops[i]
                        key = id(o.sem)
                        if key not in fin or fin[key][1] < o.val:
                            fin[key] = (o.sem, o.val)
                    for key, (sem, val) in fin.items():
                        h.wait_ge(sem, val)

            @block.tensor
            def _(e):
                run("pe", e)

            @block.scalar
            def _(e):
                run("act", e)

            @block.vector
            def _(e):
                run("dve", e)

            @block.gpsimd
            def _(e):
                run("pool", e)

            @block.sync
            def _(e):
                run("sp", e)


class Rot:
    def __init__(self, items):
        self.items = items
        self.i = 0

    def next(self):
        it = self.items[self.i % len(self.items)]
        self.i += 1
        return it


def l0_cols():
    cols = []
    for h in range(8):
        cols += [128 * h, 1024 + 128 * h, 2048 + 128 * h, 3072 + 128 * h]
    for g in range(2):
        cols += [5120 + 128 * g, 5376 + 128 * g, 5632 + 128 * g, 5888 + 128 * g,
                 6144 + 128 * g, 6400 + 128 * g]
        if g == 0:
            cols += [-1]
        cols += [4096 + 128 * (4 * g + r) for r in range(4)]
        cols += [6680 + 128 * (4 * g + r) for r in range(4)]
    return cols


def l1_cols():
    cols = []
    for j in range(8):
        cols += [128 * j, 1024 + 128 * j]
    for j in range(8):
        cols += [2048 + 128 * j]
    for hh in range(4):
        for gi in range(3):
            hd = 4 * gi + hh
            cols += [3072 + 128 * hd, 3072 + 1536 + 128 * hd, 3072 + 3072 + 128 * hd]
        cols += [3072 + 4608 + 128 * hh]
    return cols


def tile_w(w, cols):
    K = w.shape[0]
    out = np.zeros((len(cols), 128, K // 128, 128), np.float32)
    for i, c in enumerate(cols):
        if c < 0:
            blk = np.zeros((K, 128), np.float32)
            blk[:, :24] = w[:, 6656:6680]
        else:
            blk = w[:, c:c + 128]
        out[i] = blk.reshape(K // 128, 128, 128).transpose(1, 0, 2)
    return out


def host_consts():
    c = {}
    bf = ml_dtypes.bfloat16
    c["idf"] = np.eye(128, dtype=np.float32)
    c["onesb"] = np.ones((128, 128), bf)
    rp = np.zeros((128, 128), np.float32)
    for m in range(128):
        rp[(m + 64) % 128, m] = 1.0
    c["rperm"] = rp.astype(bf)
    half = 64
    inv = (10000.0 ** (-np.arange(half, dtype=np.float32) / half)).astype(np.float32)
    ang = (np.arange(S, dtype=np.float32)[None, :] * inv[:, None]).astype(np.float32)
    cos, sin = np.cos(ang), np.sin(ang)
    c["rcc"] = np.concatenate([cos, cos], 0).astype(np.float32)
    c["rss"] = np.concatenate([-sin, sin], 0).astype(np.float32)
    i = np.arange(128, dtype=np.float64)[:, None]
    j = np.arange(512, dtype=np.float64)[None, :]
    rt = np.zeros((8, 128, 5, 512), np.float32)
    for h in range(8):
        lg = math.log(1.0 - 2.0 ** (-5.0 - h))
        rt[h, :, 0, :] = np.exp((j - i) * lg)
        for r in range(4):
            e = j - i - 128 * r
            rt[h, :, 1 + r, :] = np.where(e >= 0, np.exp(np.maximum(e, 0) * lg), 0.0)
    c["rtab"] = rt
    cm = np.zeros((128, 4, 512), np.float32)
    wm = np.zeros((128, 4, 512), np.float32)
    for r in range(4):
        cm[:, r, :] = (j - i - 128 * r >= 0)
        wm[:, r, :] = (j - i - 128 * (r - 4) <= 511)
    c["cm"] = cm.astype(bf)
    c["wm"] = wm.astype(bf)
    n = np.arange(S)[None, :]
    cc = np.arange(128)[:, None]
    valid = ((16 * cc + 31 <= n) & (cc < 127)).astype(np.float32)
    c["valid"] = valid.astype(bf)
    cs = np.arange(127) * 16
    ss = np.arange(32) * 64
    ovl = np.zeros((128, 32), np.float32)
    ovl[:127] = ((cs[:, None] < ss[None, :] + 64) & (cs[:, None] + 32 > ss[None, :]))
    c["ovl"] = ovl.astype(bf)
    t = np.arange(S)
    cur = t // 64
    jj = np.arange(32)
    forced = (jj[None, :] == 0) | (jj[None, :] == cur[:, None]) | (jj[None, :] == cur[:, None] - 1)
    future = jj[None, :] > cur[:, None]
    ft = np.where(forced, 1e30, np.where(future, -1e30, 0.0)).astype(np.float32)
    c["ftab"] = np.ascontiguousarray(ft.reshape(16, 128, 32).transpose(1, 0, 2))
    ex = np.zeros((128, 16, 128), np.float32)
    for kt in range(16):
        for m in range(128):
            ex[(128 * kt + m) // 64, kt, m] = NEGB
    c["expd"] = ex.astype(bf)
    sel = np.zeros((32, 24, 128), np.float32)
    for k in range(24):
        sel[k, k, :] = 1.0
    c["sel"] = sel.astype(bf)
    m_ = np.arange(128)[:, None]
    n_ = np.arange(128)[None, :]
    prev = (m_ >= n_).astype(np.float32)
    diag = (m_ <= n_).astype(np.float32)
    dm = np.zeros((128, 2, 512), np.float32)
    dm[:, 0, :] = np.concatenate([prev, diag, prev, diag], 1)
    dm[:, 1, :] = np.concatenate([np.zeros_like(prev), diag, prev, diag], 1)
    c["dm"] = dm.astype(bf)
    return c


CONST_SPECS = {
    "idf": ([128, 128], F32), "onesb": ([128, 128], BF16), "rperm": ([128, 128], BF16),
    "rcc": ([128, S], F32), "rss": ([128, S], F32), "rtab": ([8, 128, 5, 512], F32),
    "cm": ([128, 4, 512], BF16), "wm": ([128, 4, 512], BF16), "valid": ([128, S], BF16),
    "ovl": ([128, 32], BF16), "ftab": ([128, 16, 32], F32), "expd": ([128, 16, 128], BF16),
    "sel": ([32, 24, 128], BF16), "dm": ([128, 2, 512], BF16),
}


def host_params(inp):
    p = {}
    f = np.float32
    p["wl0"] = tile_w(np.asarray(inp["ev_w_in"][0], f), l0_cols())
    p["wl1"] = tile_w(np.asarray(inp["od_w_in"][0], f), l1_cols())

    def wo_l(w):
        K = w.shape[0]
        return np.ascontiguousarray(w.reshape(K // 128, 128, 4, 512).transpose(2, 1, 0, 3))

    p["wo0"] = wo_l(np.asarray(inp["ev_w_out"][0], f))
    p["wo1"] = wo_l(np.asarray(inp["od_w_out"][0], f))
    p["g0"] = np.ascontiguousarray(np.asarray(inp["ev_norm"][0], f).reshape(16, 128).T)
    p["g1"] = np.ascontiguousarray(np.asarray(inp["od_norm"][0], f).reshape(16, 128).T)
    p["retg"] = np.ascontiguousarray(np.asarray(inp["ev_ret_norm"][0], f).T)
    p["nqg"] = np.ascontiguousarray(np.asarray(inp["ev_nsa_q_norm"][0], f).reshape(128, 1))
    p["nkg"] = np.ascontiguousarray(np.asarray(inp["ev_nsa_k_norm"][0], f).T)
    p["posk"] = np.ascontiguousarray(np.asarray(inp["ev_cmp_pos_k"][0], f).T)
    p["posv"] = np.ascontiguousarray(np.asarray(inp["ev_cmp_pos_v"][0], f).T)
    p["w1k"] = np.ascontiguousarray(np.asarray(inp["ev_cmp_k_w1"][0], f).reshape(32, 128, 128).transpose(1, 0, 2))
    p["w1v"] = np.ascontiguousarray(np.asarray(inp["ev_cmp_v_w1"][0], f).reshape(32, 128, 128).transpose(1, 0, 2))
    p["w2k"] = np.ascontiguousarray(np.asarray(inp["ev_cmp_k_w2"][0], f))
    p["w2v"] = np.ascontiguousarray(np.asarray(inp["ev_cmp_v_w2"][0], f))
    p["dww"] = np.ascontiguousarray(np.asarray(inp["od_dw_w"][0], f).reshape(31, 8, 128).transpose(2, 1, 0))
    p["dwb"] = np.ascontiguousarray(np.asarray(inp["od_dw_b"][0], f).reshape(8, 128).T)
    p["cng"] = np.ascontiguousarray(np.asarray(inp["od_conv_norm_g"][0], f).reshape(8, 128).T)
    p["cnb"] = np.ascontiguousarray(np.asarray(inp["od_conv_norm_b"][0], f).reshape(8, 128).T)
    p["dqg"] = np.ascontiguousarray(np.asarray(inp["od_dil_q_norm"][0], f).reshape(128, 1))
    p["dkg"] = np.ascontiguousarray(np.asarray(inp["od_dil_k_norm"][0], f).reshape(128, 1))
    return p


PARAM_SPECS = {
    "wl0": [61, 128, 16, 128], "wl1": [64, 128, 16, 128], "wo0": [4, 128, 16, 512], "wo1": [4, 128, 12, 512],
    "g0": [128, 16], "g1": [128, 16], "retg": [128, 8], "nqg": [128, 1], "nkg": [128, 3],
    "posk": [128, 32], "posv": [128, 32], "w1k": [128, 32, 128], "w1v": [128, 32, 128],
    "w2k": [128, 128], "w2v": [128, 128], "dww": [128, 8, 31], "dwb": [128, 8], "cng": [128, 8],
    "cnb": [128, 8], "dqg": [128, 1], "dkg": [128, 1],
}


import os
ARW = int(os.environ.get("ARW", "88064"))
NW = int(os.environ.get("NW", "4"))
XNT_END = 16 * S


class Arena:
    def __init__(self, ap):
        self.ap = ap
        self.off = 0

    def reset(self, off=0):
        if os.environ.get("ARDBG") and self.off:
            print("arena phase end off", self.off)
        self.off = off

    def take(self, n, dt):
        nb = n * 2 if dt == F32 else n
        nb += nb % 2
        assert self.off + nb <= ARW, ("arena overflow", self.off, nb)
        a = self.ap[:, self.off:self.off + nb]
        self.off += nb
        if dt == F32:
            a = a.bitcast(F32)
        return a[:, 0:n]


def build(nseq=2, layers=(0, 1), dbg=False, phases=None):
    nc = bass.Bass("TRN2", target_bir_lowering=False)
    NT = nseq * S
    dr = {}
    x_d = nc.dram_tensor("x", [NT, D], F32, kind="ExternalInput").ap()
    out_d = nc.dram_tensor("out", [NT, D], F32, kind="ExternalOutput").ap()
    for k, (shp, dt) in CONST_SPECS.items():
        dr[k] = nc.dram_tensor("c_" + k, shp, dt, kind="ExternalInput").ap()
    for k, shp in PARAM_SPECS.items():
        dr[k] = nc.dram_tensor("p_" + k, shp, F32, kind="ExternalInput").ap()
    x1_d = nc.dram_tensor("x1s", [NT, D], F32, kind="Internal").ap()
    mix_d = nc.dram_tensor("mixs", [nseq * 2, 16, 128, S], BF16,
                           kind="ExternalOutput" if dbg else "Internal").ap()

    es = ExitStack()
    with es:
        def sb(name, shape, dt):
            return es.enter_context(nc.sbuf_tensor(name, shape, dt))

        ARt = sb("arena", [128, ARW], BF16)
        WBt = sb("wbr", [128, NW * 2048], BF16)
        IDF = sb("idf", [128, 128], F32)
        IDB = sb("idb", [128, 128], BF16)
        ONESB = sb("onesb", [128, 128], BF16)
        RPERM = sb("rperm", [128, 128], BF16)
        PRM = sb("prm", [128, 128], F32)
        psb = [es.enter_context(nc.psum_tensor("psb%d" % i, [128, 512], F32)) for i in range(8)]
        PJ = Rot([(psb[0], "PJ0"), (psb[1], "PJ1"), (psb[6], "ST2")] if os.environ.get("PJ3", "0") == "1"
                 else [(psb[0], "PJ0"), (psb[1], "PJ1")])
        ST = Rot([(psb[2], "ST0"), (psb[3], "ST1"), (psb[6], "ST2")])
        OA = Rot([(psb[4], "OA0"), (psb[5], "OA1")])
        LA = Rot([(psb[7], "LA0")])
        SKEW = int(os.environ.get("SKEW", "2"))
        ar = Arena(ARt[:, :])
        WB = WBt[:, :].rearrange("p (s k c) -> p s k c", s=NW, k=16)
        P = Prog(nc)

        def mm(out, lhsT, rhs, start, stop, reads, writes):
            P.pe(lambda e: e.matmul(out, lhsT=lhsT, rhs=rhs, start=start, stop=stop), reads, writes)

        def TR(out, in_, reads, writes):
            P.pe(lambda e: e.transpose(out=out, in_=in_, identity=IDF[:, :]), list(reads) + ["IDF"], writes)

        def ACT(out, in_, func, reads, writes, scale=None, bias=None, accum=None):
            kw = {}
            if scale is not None:
                kw["scale"] = scale
            if bias is not None:
                kw["bias"] = bias
            if accum is not None:
                kw["accum_out"] = accum
            P.act(lambda e: e.activation(out=out, in_=in_, func=func, **kw), reads, writes)

        def TT(out, in0, in1, op, reads, writes):
            P.dve(lambda e: e.tensor_tensor(out=out, in0=in0, in1=in1, op=op), reads, writes)

        def PTT(out, in0, in1, op, reads, writes):
            P.pool(lambda e: e.tensor_tensor(out=out, in0=in0, in1=in1, op=op), reads, writes)

        def TS(out, in0, s1, s2, op0, op1, reads, writes):
            if op1 is None:
                P.dve(lambda e: e.tensor_scalar(out=out, in0=in0, scalar1=s1, scalar2=None, op0=op0), reads, writes)
            else:
                P.dve(lambda e: e.tensor_scalar(out=out, in0=in0, scalar1=s1, scalar2=s2, op0=op0, op1=op1), reads, writes)

        def STT(out, in0, scalar, in1, op0, op1, reads, writes):
            P.dve(lambda e: e.scalar_tensor_tensor(out=out, in0=in0, scalar=scalar, in1=in1, op0=op0, op1=op1), reads, writes)

        def RECIP(out, in_, reads, writes):
            P.dve(lambda e: e.reciprocal(out=out, in_=in_), reads, writes)

        def DMA(eng, out, in_, reads, writes, is_out=False):
            P.dma(eng, lambda e: e.dma_start(out=out, in_=in_), reads, writes, is_out=is_out)

        MUL, ADD, SUB = ALU.mult, ALU.add, ALU.subtract

        DMA("sp", IDF[:, :], dr["idf"][:, :], [], ["IDF"])
        DMA("sp", ONESB[:, :], dr["onesb"][:, :], [], ["ONESB"])
        DMA("sp", RPERM[:, :], dr["rperm"][:, :], [], ["RPERM"])
        ACT(IDB[:, :], IDF[:, :], AF.Copy, ["IDF"], ["IDB"])
        pcol = {"g0": (0, 16), "g1": (16, 16), "retg": (32, 8), "nqg": (40, 1), "nkg": (41, 3),
                "dwb": (44, 8), "cng": (52, 8), "cnb": (60, 8), "dqg": (68, 1), "dkg": (69, 1)}
        for k, (c0, n) in pcol.items():
            DMA("sp", PRM[:, c0:c0 + n], dr[k][:, :], [], ["PRM"])

        def prm(k, j=0, n=1):
            c0 = pcol[k][0] + j
            return PRM[:, c0:c0 + n]

        wlist = []
        for seq in range(nseq):
            for ly in layers:
                nt = 61 if ly == 0 else 64
                for i in range(nt):
                    wlist.append(dr["wl%d" % ly][i])
        wstate = {"next": 0, "loaded": 0}

        def next_w():
            i = wstate["next"]
            wstate["next"] += 1
            upto = min(i + NW - 2, len(wlist) - 1)
            while wstate["loaded"] <= upto:
                j = wstate["loaded"]
                s = j % NW
                DMA("pool", WB[:, s], wlist[j], [], ["WB%d" % s])
                wstate["loaded"] += 1
            s = i % NW
            return WB[:, s], "WB%d" % s

        XNT = None

        def proj_fm(w, tb, M=128):
            wa, wt = w
            ps, pt = PJ.next()
            for kc in range(16):
                mm(ps[0:M, :], wa[:, kc, 0:M], XNT[:, kc, tb * 512:(tb + 1) * 512], kc == 0, kc == 15,
                   [wt, "XNT"], [pt])
            return ps, pt

        def proj_tm(w, tok_aps):
            wa, wt = w
            ps, pt = PJ.next()
            for j, tap in enumerate(tok_aps):
                for kc in range(16):
                    mm(ps[:, j * 128:(j + 1) * 128], tap(kc), wa[:, kc, :], kc == 0, kc == 15, [wt, "XNT"], [pt])
            return ps, pt

        def nat_tok(tt):
            return lambda kc: XNT[:, kc, tt * 128:(tt + 1) * 128]

        def phaseA(xin_d, seq, gname):
            ar.reset(XNT_END)
            XT = [ar.take(D, F32) for _ in range(2)]
            XSs = [ar.take(D, F32) for _ in range(2)]
            JUNK = ar.take(D, BF16)
            stts = [ar.take(4, F32) for _ in range(2)]
            for tt in range(16):
                xt = XT[tt % 2]
                xk = "XT%d" % (tt % 2)
                XS = XSs[tt % 2]; xsk = "XS%d" % (tt % 2)
                stt = stts[tt % 2]; sk_ = "st%d" % (tt % 2)
                r0 = seq * S + tt * 128
                DMA("sp", xt, xin_d[r0:r0 + 128, :], [], [xk])
                ACT(JUNK, xt, AF.Square, [xk], ["JUNK", sk_], accum=stt[:, 0:1])
                TS(stt[:, 1:2], stt[:, 0:1], 1.0 / D, EPS, MUL, ADD, [sk_], [sk_])
                ACT(stt[:, 1:2], stt[:, 1:2], AF.Ln, [sk_], [sk_])
                ACT(stt[:, 2:3], stt[:, 1:2], AF.Exp, [sk_], [sk_], scale=-0.5)
                ACT(XS, xt, AF.Copy, [xk, sk_], [xsk], scale=stt[:, 2:3])
                for q in range(4):
                    ps, pt = PJ.next()
                    for j in range(4):
                        kc = 4 * q + j
                        TR(ps[:, j * 128:(j + 1) * 128], XS[:, kc * 128:(kc + 1) * 128], [xsk], [pt])
                    TT(XNT[:, 4 * q:4 * q + 4, tt * 128:(tt + 1) * 128],
                       ps[:, :].rearrange("p (a b) -> p a b", a=4),
                       prm(gname, 4 * q, 4).unsqueeze(2).to_broadcast([128, 4, 128]), MUL,
                       [pt, "PRM"], ["XNT"])

        def norm_fm(ps, pt, N, gain, out, outtok, SQ, RS):
            ACT(SQ[:, 0:N], ps[:, 0:N], AF.Square, [pt], ["SQ"])
            la, lat = LA.next()
            mm(la[:, 0:N], ONESB[:, :], SQ[:, 0:N], True, True, ["SQ", "ONESB"], [lat])
            ACT(RS[:, 0:N], la[:, 0:N], AF.Ln, [lat], ["RS"], scale=1.0 / 128, bias=EPS)
            ACT(RS[:, 0:N], RS[:, 0:N], AF.Exp, ["RS"], ["RS"], scale=-0.5)
            STT(out, ps[:, 0:N], gain, RS[:, 0:N], MUL, MUL, [pt, "RS", "PRM"], [outtok])

        def retention(seq, mixd):
            ar.reset(XNT_END)
            QT = ar.take(S, BF16); KT = ar.take(S, BF16); ZS = ar.take(S, BF16)
            VT = ar.take(S, BF16).rearrange("p (t e) -> p t e", t=16)
            RCC = ar.take(S, F32); RSS = ar.take(S, F32)
            RT = [ar.take(5 * 512, F32).rearrange("p (v n) -> p v n", v=5) for _ in range(2)]
            QRAW = ar.take(512, BF16)
            TA = ar.take(512, F32); TB = ar.take(512, F32)
            PTs = [ar.take(512, BF16) for _ in range(4)]
            PTR = Rot([(PTs[i], "PT%d" % i) for i in range(4)])
            SQ = ar.take(512, BF16); RS = ar.take(512, F32)
            MH = [ar.take(S, BF16) for _ in range(2)]
            DMA("sp", RCC, dr["rcc"][:, :], [], ["RCC"])
            DMA("sp", RSS, dr["rss"][:, :], [], ["RSS"])
            for h in range(int(os.environ.get('RET_HEADS', '8'))):
                gam = 1.0 - 2.0 ** (-5.0 - h)
                rt = RT[h % 2]; rtk = "RT%d" % (h % 2)
                if h == 0:
                    DMA("sp", rt, dr["rtab"][0], [], [rtk])
                if h + 1 < int(os.environ.get('RET_HEADS', '8')):
                    DMA("sp", RT[(h + 1) % 2], dr["rtab"][h + 1], [], ["RT%d" % ((h + 1) % 2)])
                RSTOP = float(os.environ.get("RET_STOP", "99"))
                if RSTOP <= 1:
                    continue
                for (dst, dtok) in ((QT, "QT"), (KT, "KT")):
                    w = next_w()
                    for tb in range(4):
                        blk = slice(tb * 512, (tb + 1) * 512)
                        if RSTOP <= 1.2:
                            continue
                        ps, pt = proj_fm(w, tb)
                        ACT(QRAW, ps[:, :], AF.Copy, [pt], ["QRAW"])
                        if RSTOP <= 1.5:
                            continue
                        ps2, pt2 = ST.next()
                        mm(ps2[:, :], RPERM[:, :], QRAW, True, True, ["QRAW", "RPERM"], [pt2])
                        if RSTOP <= 1.7:
                            continue
                        if os.environ.get("VARA"):
                            TT(TA, QRAW, RCC[:, blk], MUL, ["QRAW", "RCC"], ["TA"])
                        else:
                            TT(TA, ps[:, :], RCC[:, blk], MUL, [pt, "RCC"], ["TA"])
                        if RSTOP <= 1.8:
                            continue
                        TT(TB, ps2[:, :], RSS[:, blk], MUL, [pt2, "RSS"], ["TB"])
                        if RSTOP <= 1.9:
                            continue
                        TT(dst[:, blk], TA, TB, ADD, ["TA", "TB"], [dtok])
                if RSTOP <= 2:
                    continue
                w = next_w()
                for t4 in range(4):
                    ps, pt = proj_tm(w, [nat_tok(4 * t4 + j) for j in range(4)])
                    ACT(VT[:, 4 * t4:4 * t4 + 4, :], ps[:, :].rearrange("p (a b) -> p a b", a=4), AF.Copy, [pt], ["VT"])
                if RSTOP <= 3:
                    continue
                w = next_w()
                for tb in range(4):
                    ps, pt = proj_fm(w, tb)
                    ACT(ZS[:, tb * 512:(tb + 1) * 512], ps[:, :], AF.Silu, [pt], ["ZS"])
                if RSTOP <= 4:
                    continue
                mh = MH[h % 2]; mhk = "MH%d" % (h % 2)
                for tb in range(4):
                    blk = slice(tb * 512, (tb + 1) * 512)
                    nk = 4 * tb + 4
                    oa, oat = OA.next()

                    def pv(pr, nk=nk, oa=oa, oat=oat):
                        kt, pa, pk = pr
                        mm(oa[:, :], VT[:, kt, :], pa, kt == 0, kt == nk - 1, ["VT", pk], [oat])

                    pend = []
                    for kt in range(nk):
                        st, sk = ST.next()
                        mm(st[:, :], KT[:, kt * 128:(kt + 1) * 128], QT[:, blk], True, True, ["KT", "QT"], [sk])
                        if len(pend) >= SKEW:
                            pv(pend.pop(0))
                        r = kt - 4 * tb
                        pa, pk = PTR.next()
                        if r < 0:
                            STT(pa, st[:, :], float(gam ** (-128 * r) * SCALE), rt[:, 0, :], MUL, MUL, [sk, rtk], [pk])
                        else:
                            STT(pa, st[:, :], float(SCALE), rt[:, 1 + r, :], MUL, MUL, [sk, rtk], [pk])
                        pend.append((kt, pa, pk))
                    for pr in pend:
                        pv(pr)
                    ACT(SQ, oa[:, :], AF.Square, [oat], ["SQ"])
                    la, lat = LA.next()
                    mm(la[:, :], ONESB[:, :], SQ, True, True, ["SQ", "ONESB"], [lat])
                    ACT(RS, la[:, :], AF.Ln, [lat], ["RS"], scale=1.0 / 128, bias=EPS)
                    ACT(RS, RS, AF.Exp, ["RS"], ["RS"], scale=-0.5)
                    STT(TA, oa[:, :], prm("retg", h), RS, MUL, MUL, [oat, "RS", "PRM"], ["TA"])
                    TT(mh[:, blk], TA, ZS[:, blk], MUL, ["TA", "ZS"], [mhk])
                DMA("sp", mixd[h], mh, [mhk], ["mixd"])

        def nsa(seq, mixd):
            ar.reset(XNT_END)
            KZ = ar.take(S, BF16)
            VB = ar.take(S, BF16)
            KST = ar.take(S, BF16); KWT = ar.take(S, BF16)
            VS = ar.take(S, BF16).rearrange("p (t e) -> p t e", t=16)
            VW = ar.take(S, BF16).rearrange("p (t e) -> p t e", t=16)
            QN = ar.take(4 * S, BF16).rearrange("p (r n) -> p r n", r=4)
            SG = ar.take(S, BF16)
            W1K = ar.take(4096, BF16).rearrange("p (l e) -> p l e", l=32)
            W1V = ar.take(4096, BF16).rearrange("p (l e) -> p l e", l=32)
            W2K = ar.take(128, BF16); W2V = ar.take(128, BF16)
            POSK = ar.take(32, BF16); POSV = ar.take(32, BF16)
            GG = ar.take(128, BF16); KCN = ar.take(128, BF16); VC = ar.take(128, BF16)
            CM = ar.take(2048, BF16).rearrange("p (r n) -> p r n", r=4)
            WM = ar.take(2048, BF16).rearrange("p (r n) -> p r n", r=4)
            VALID = ar.take(S, BF16)
            OVL = ar.take(32, BF16)
            FTAB = ar.take(512, F32).rearrange("p (t j) -> p t j", t=16)
            EXPD = ar.take(2048, BF16).rearrange("p (t m) -> p t m", t=16)
            SEL = ar.take(24 * 128, BF16).rearrange("p (c m) -> p c m", c=24)
            PTs = [ar.take(512, BF16) for _ in range(4)]
            PTR = Rot([(PTs[i], "PT%d" % i) for i in range(4)])
            PN = ar.take(512, BF16)
            SQ = ar.take(512, BF16); RS = ar.take(512, F32)
            RL = ar.take(512, F32); CF = ar.take(512, F32); ACC = ar.take(512, F32); TMP = ar.take(512, F32)
            U = ar.take(128, F32); U2 = ar.take(128, F32); B1 = ar.take(2, F32)
            IA4 = ar.take(128, F32).rearrange("p (j c) -> p j c", j=4); IB = ar.take(32, F32); M8 = ar.take(8, F32); M8b = ar.take(8, F32)
            SELM = ar.take(32, F32)
            MH = [ar.take(S, BF16)] * 2
            for (dst, key, tok) in ((CM, "cm", "CM"), (WM, "wm", "WM"), (EXPD, "expd", "EXPD"), (SEL, "sel", "SEL"),
                                    (FTAB, "ftab", "FTAB")):
                src = dr[key]
                if key in ("expd", "sel"):
                    DMA("sp", dst[0:32] if key == "sel" else dst, src, [], [tok])
                else:
                    DMA("sp", dst, src, [], [tok])
            DMA("sp", VALID, dr["valid"][:, :], [], ["VALID"])
            DMA("sp", OVL, dr["ovl"][:, :], [], ["OVL"])
            DMA("pool", W1K, dr["w1k"], [], ["W1K"])
            DMA("pool", W1V, dr["w1v"], [], ["W1V"])
            DMA("pool", W2K, dr["w2k"][:, :], [], ["W2K"])
            DMA("pool", W2V, dr["w2v"][:, :], [], ["W2V"])
            DMA("pool", POSK, dr["posk"][:, :], [], ["POSK"])
            DMA("pool", POSV, dr["posv"][:, :], [], ["POSV"])

            def compress(raw, rawtok, W1, w1t, POS, post, W2, w2t, is_k):
                pb, pbt = PJ.next()
                for l in range(32):
                    mm(pb[:, 0:1], W1[:, l, :], POS[:, l:l + 1], l == 0, l == 31, [w1t, post], [pbt])
                ACT(B1[:, 0:1], pb[:, 0:1], AF.Copy, [pbt], ["B1"])
                ph, pht = PJ.next()
                rv = raw.rearrange("p (c s) -> p c s", s=16)
                for l in range(32):
                    rhs = rv[:, 0:127, l] if l < 16 else rv[:, 1:128, l - 16]
                    mm(ph[:, 0:127], W1[:, l, :], rhs, l == 0, l == 31, [w1t, rawtok], [pht])
                u, u2 = U[:, 0:127], U2[:, 0:127]
                TS(u, ph[:, 0:127], B1[:, 0:1], None, ADD, None, [pht, "B1"], ["U"])
                TT(u2, u, u, MUL, ["U"], ["U2"])
                TS(u2, u2, 0.044715, 1.0, MUL, ADD, ["U2"], ["U2"])
                TT(u2, u2, u, MUL, ["U2", "U"], ["U2"])
                ACT(u2, u2, AF.Sigmoid, ["U2"], ["U2"], scale=1.5957691216057308)
                TT(GG[:, 0:127], u, u2, MUL, ["U", "U2"], ["GG"])
                if is_k:
                    pk_, pkt = PJ.next()
                    mm(pk_[:, 0:127], W2, GG[:, 0:127], True, True, [w2t, "GG"], [pkt])
                    norm_fm(pk_, pkt, 127, prm("nkg", 0), KCN[:, 0:127], "KCN", SQ, RS)
                else:
                    pv_, pvt = PJ.next()
                    mm(pv_[0:127, 0:128], GG[:, 0:127], W2, True, True, [w2t, "GG"], [pvt])
                    ACT(VC[0:127, :], pv_[0:127, 0:128], AF.Copy, [pvt], ["VC"])

            def attn(qap, tiles, oa, oat, la, lat):
                n = len(tiles)

                def pv(pr):
                    i, t, pa, pk = pr
                    nk = t["nk"]
                    mm(oa[:, :], t["v"][0], pa[0:nk, :], i == 0, i == n - 1, [t["v"][1], pk], [oat])
                    mm(la[:, :], ONESB[0:nk, :], pa[0:nk, :], i == 0, i == n - 1, ["ONESB", pk], [lat])

                pend = []
                for i, t in enumerate(tiles):
                    st, sk = ST.next()
                    nk = t["nk"]
                    b = t.get("bias")
                    mm(st[0:nk, :], t["k"][0], qap, True, b is None, [t["k"][1], "QN"], [sk])
                    if b is not None:
                        mm(st[0:nk, :], b[0], b[1], False, True, ["EXPD", "VB"], [sk])
                    if len(pend) >= SKEW:
                        pv(pend.pop(0))
                    pa, pk = PTR.next()
                    ACT(pa[0:nk, :], st[0:nk, :], AF.Exp, [sk], [pk], scale=float(SCALE))
                    m = t.get("mask")
                    if m is not None:
                        if t.get("pool") and os.environ.get("POOLMASK", "1") == "1":
                            PTT(pa[0:nk, :], pa[0:nk, :], m[0], MUL, [pk, m[1]], [pk])
                        else:
                            TT(pa[0:nk, :], pa[0:nk, :], m[0], MUL, [pk, m[1]], [pk])
                    pend.append((i, t, pa, pk))
                for pr in pend:
                    pv(pr)

            def branch_fin(oa, oat, la, lat, gcol, blk, first):
                pg, pgt = PJ.next()
                mm(pg[:, :], SEL[0:32, gcol, :], SG[0:32, blk], True, True, ["SEL", "SG"], [pgt])
                if first:
                    ACT(RL, la[:, :], AF.Ln, [lat], ["RL"], bias=1e-18)
                else:
                    ACT(RL, la[:, :], AF.Ln, [lat], ["RL"])
                ACT(RL, RL, AF.Exp, ["RL"], ["RL"], scale=-1.0)
                TT(CF, RL, pg[:, :], MUL, ["RL", pgt], ["CF"])
                if first:
                    TT(ACC, oa[:, :], CF, MUL, [oat, "CF"], ["ACC"])
                else:
                    TT(TMP, oa[:, :], CF, MUL, [oat, "CF"], ["TMP"])
                    TT(ACC, ACC, TMP, ADD, ["ACC", "TMP"], ["ACC"])

            for g in range(2):
                w = next_w()
                for tb in range(4):
                    ps, pt = proj_fm(w, tb)
                    ACT(KZ[:, tb * 512:(tb + 1) * 512], ps[:, :], AF.Copy, [pt], ["KZ"])
                w = next_w()
                for tb in range(4):
                    ps, pt = proj_fm(w, tb)
                    ACT(VB[:, tb * 512:(tb + 1) * 512], ps[:, :], AF.Copy, [pt], ["VB"])
                w = next_w()
                for tb in range(4):
                    ps, pt = proj_fm(w, tb)
                    norm_fm(ps, pt, 512, prm("nkg", 1), KST[:, tb * 512:(tb + 1) * 512], "KST", SQ, RS)
                w = next_w()
                for t4 in range(4):
                    ps, pt = proj_tm(w, [nat_tok(4 * t4 + j) for j in range(4)])
                    ACT(VS[:, 4 * t4:4 * t4 + 4, :], ps[:, :].rearrange("p (a b) -> p a b", a=4), AF.Copy, [pt], ["VS"])
                w = next_w()
                for tb in range(4):
                    ps, pt = proj_fm(w, tb)
                    norm_fm(ps, pt, 512, prm("nkg", 2), KWT[:, tb * 512:(tb + 1) * 512], "KWT", SQ, RS)
                w = next_w()
                for t4 in range(4):
                    ps, pt = proj_tm(w, [nat_tok(4 * t4 + j) for j in range(4)])
                    ACT(VW[:, 4 * t4:4 * t4 + 4, :], ps[:, :].rearrange("p (a b) -> p a b", a=4), AF.Copy, [pt], ["VW"])
                if g == 0:
                    w = next_w()
                    for tb in range(4):
                        ps, pt = proj_fm(w, tb, M=32)
                        ACT(SG[0:32, tb * 512:(tb + 1) * 512], ps[0:32, :], AF.Sigmoid, [pt], ["SG"])
                compress(KZ, "KZ", W1K, "W1K", POSK, "POSK", W2K, "W2K", True)
                compress(VB, "VB", W1V, "W1V", POSV, "POSV", W2V, "W2V", False)
                for r in range(4):
                    w = next_w()
                    for tb in range(4):
                        ps, pt = proj_fm(w, tb)
                        norm_fm(ps, pt, 512, prm("nqg", 0), QN[:, r, tb * 512:(tb + 1) * 512], "QN", SQ, RS)
                for tb in (2, 3):
                    blk = slice(tb * 512, (tb + 1) * 512)
                    pim, pimt = PJ.next()
                    pns = []
                    for r in range(4):
                        st, sk = ST.next()
                        mm(st[0:127, :], KCN[:, 0:127], QN[:, r, blk], True, True, ["KCN", "QN"], [sk])
                        pa, pk = PTR.next()
                        ACT(pa[0:127, :], st[0:127, :], AF.Exp, [sk], [pk], scale=float(SCALE))
                        TT(pa[0:127, :], pa[0:127, :], VALID[0:127, blk], MUL, [pk, "VALID"], [pk])
                        la, lat = LA.next()
                        mm(la[:, :], ONESB[0:127, :], pa[0:127, :], True, True, ["ONESB", pk], [lat])
                        ACT(RL, la[:, :], AF.Ln, [lat], ["RL"], bias=1e-18)
                        ACT(RL, RL, AF.Exp, ["RL"], ["RL"], scale=-1.0)
                        TT(pa[0:127, :], pa[0:127, :], RL[0:127, :], MUL, [pk, "RL"], [pk])
                        pns.append((pa, pk))
                    for j in range(4):
                        for r in range(4):
                            pa, pk = pns[r]
                            mm(pim[:, j * 32:(j + 1) * 32], pa[0:127, j * 128:(j + 1) * 128], OVL[0:127, :],
                               r == 0, r == 3, [pk, "OVL"], [pimt])
                    TT(IA4, pim[:, 0:128].rearrange("p (j c) -> p j c", j=4), FTAB[:, 4 * tb:4 * tb + 4, :], ADD,
                       [pimt, "FTAB"], ["IA"])
                    for j in range(4):
                        tt = 4 * tb + j
                        IA = IA4[:, j, :]
                        P.dve(lambda e, IA=IA: e.max(out=M8, in_=IA), ["IA"], ["M8"])
                        P.dve(lambda e, IA=IA: e.match_replace(out=IB, in_to_replace=M8, in_values=IA, imm_value=-3.0e38),
                              ["IA", "M8"], ["IB"])
                        P.dve(lambda e: e.max(out=M8b, in_=IB), ["IB"], ["M8b"])
                        TS(SELM, IA, M8b[:, 7:8], 1.0, ALU.is_ge, SUB, ["IA", "M8b"], ["SELM"])
                        ptp, ptpt = ST.next()
                        TR(ptp[0:32, 0:128], SELM, ["SELM"], [ptpt])
                        ACT(VB[0:32, tt * 128:(tt + 1) * 128], ptp[0:32, 0:128], AF.Copy, [ptpt], ["VB"])
                for r in range(4):
                    h = 4 * g + r
                    w = next_w()
                    for tb in range(4):
                        ps, pt = proj_fm(w, tb)
                        ACT(KZ[:, tb * 512:(tb + 1) * 512], ps[:, :], AF.Silu, [pt], ["KZ"])
                    mh = MH[0]; mhk = "MH0"
                    for tb in range(4):
                        blk = slice(tb * 512, (tb + 1) * 512)
                        qap = QN[:, r, blk]
                        oa, oat = OA.next(); la, lat = LA.next()
                        attn(qap, [dict(k=(KCN[:, 0:127], "KCN"), nk=127, v=(VC[0:127, :], "VC"),
                                        mask=(VALID[0:127, blk], "VALID"))], oa, oat, la, lat)
                        branch_fin(oa, oat, la, lat, 0 * 8 + h, blk, True)
                        tiles = []
                        for kt in range(4 * tb + 4):
                            t = dict(k=(KST[:, kt * 128:(kt + 1) * 128], "KST"), nk=128, v=(VS[:, kt, :], "VS"))
                            if tb >= 2:
                                t["bias"] = (EXPD[:, kt, :], VB[:, blk])
                            rr = kt - 4 * tb
                            if rr >= 0:
                                t["mask"] = (CM[:, rr, :], "CM")
                            tiles.append(t)
                        oa, oat = OA.next(); la, lat = LA.next()
                        attn(qap, tiles, oa, oat, la, lat)
                        branch_fin(oa, oat, la, lat, 1 * 8 + h, blk, False)
                        tiles = []
                        for kt in range(max(0, 4 * tb - 4), 4 * tb + 4):
                            rr = kt - 4 * tb
                            m = (CM[:, rr, :], "CM") if rr >= 0 else (WM[:, rr + 4, :], "WM")
                            tiles.append(dict(k=(KWT[:, kt * 128:(kt + 1) * 128], "KWT"), nk=128, v=(VW[:, kt, :], "VW"), mask=m,
                                              pool=(kt % 2 == 0)))
                        oa, oat = OA.next(); la, lat = LA.next()
                        attn(qap, tiles, oa, oat, la, lat)
                        branch_fin(oa, oat, la, lat, 2 * 8 + h, blk, False)
                        TT(mh[:, blk], ACC, KZ[:, blk], MUL, ["ACC", "KZ"], [mhk])
                    DMA("sp", mixd[8 + h], mh, [mhk], ["mixd"])

        def conv_branch(seq, mixd):
            ar.reset(XNT_END)
            A = ar.take(8 * S, F32).rearrange("p (j n) -> p j n", j=8)
            A0 = ar.take(S + 32, BF16)
            DG = ar.take(31 * 128, BF16).rearrange("p (k m) -> p k m", k=31)
            DWW = ar.take(8 * 31, F32).rearrange("p (j k) -> p j k", j=8)
            SGT = ar.take(512, F32)
            MU = ar.take(S, F32); RSTD = ar.take(S, F32)
            TBF = [ar.take(512, BF16) for _ in range(2)]
            TSQ = [ar.take(512, BF16) for _ in range(2)]
            TV = SGT; T1 = ar.take(512, F32); T2 = ar.take(512, F32)
            ZSb = ar.take(512, BF16)
            MH = [ar.take(S, BF16)] * 2
            DMA("sp", DWW, dr["dww"], [], ["DWW"])
            P.dve(lambda e: e.memset(A0[:, 0:32], 0.0), [], ["A0"])
            for j in range(8):
                wu = next_w(); wg = next_w()
                TT(DG[:, :, :], IDB[:, :].unsqueeze(1).to_broadcast([128, 31, 128]),
                   DWW[:, j, :].unsqueeze(2).to_broadcast([128, 31, 128]), MUL, ["IDB", "DWW"], ["DG"])
                for tb in range(4):
                    pu, put = proj_fm(wu, tb)
                    pg, pgt = proj_fm(wg, tb)
                    ACT(SGT, pg[:, :], AF.Sigmoid, [pgt], ["SGT"])
                    TT(A0[:, 32 + tb * 512:32 + (tb + 1) * 512], pu[:, :], SGT, MUL, [put, "SGT"], ["A0"])
                for tb in range(4):
                    pc, pct = ST.next()
                    for k in range(31):
                        mm(pc[:, :], DG[:, k, :], A0[:, 2 + k + tb * 512:2 + k + (tb + 1) * 512], k == 0, k == 30,
                           ["DG", "A0"], [pct])
                    ACT(A[:, j, tb * 512:(tb + 1) * 512], pc[:, :], AF.Identity, [pct, "PRM"], ["A"], bias=prm("dwb", j))
            for tb in range(4):
                blk = slice(tb * 512, (tb + 1) * 512)
                s1, s1t = LA.next(); s2, s2t = OA.next()
                for j in range(8):
                    ACT(TBF[j % 2], A[:, j, blk], AF.Copy, ["A"], ["TBF%d" % (j % 2)])
                    ACT(TSQ[j % 2], A[:, j, blk], AF.Square, ["A"], ["TSQ%d" % (j % 2)])
                    mm(s1[:, :], ONESB[:, :], TBF[j % 2], j == 0, j == 7, ["ONESB", "TBF%d" % (j % 2)], [s1t])
                    mm(s2[:, :], ONESB[:, :], TSQ[j % 2], j == 0, j == 7, ["ONESB", "TSQ%d" % (j % 2)], [s2t])
                TS(MU[:, blk], s1[:, :], 1.0 / 1024, None, MUL, None, [s1t], ["MU"])
                TT(TV, MU[:, blk], MU[:, blk], MUL, ["MU"], ["SGT"])
                STT(TV, s2[:, :], 1.0 / 1024, TV, MUL, SUB, [s2t, "SGT"], ["SGT"])
                ACT(TV, TV, AF.Ln, ["SGT"], ["SGT"], bias=EPS)
                ACT(RSTD[:, blk], TV, AF.Exp, ["SGT"], ["RSTD"], scale=-0.5)
            for j in range(8):
                wz = next_w()
                mh = MH[0]; mhk = "MH0"
                for tb in range(4):
                    blk = slice(tb * 512, (tb + 1) * 512)
                    pz, pzt = proj_fm(wz, tb)
                    ACT(ZSb, pz[:, :], AF.Silu, [pzt], ["ZSb"])
                    TT(T1, A[:, j, blk], MU[:, blk], SUB, ["A", "MU"], ["T1"])
                    TT(T1, T1, RSTD[:, blk], MUL, ["T1", "RSTD"], ["T1"])
                    ACT(T2, T1, AF.Silu, ["T1", "PRM"], ["T2"], scale=prm("cng", j), bias=prm("cnb", j))
                    TT(mh[:, blk], T2, ZSb, MUL, ["T2", "ZSb"], [mhk])
                DMA("sp", mixd[j], mh, [mhk], ["mixd"])

        def dilated(seq, mixd):
            ar.reset(XNT_END)
            QN = ar.take(S, BF16); KN = ar.take(S, BF16)
            VD = ar.take(S, BF16).rearrange("p (t e) -> p t e", t=16)
            OACC = ar.take(S, F32); LACC = ar.take(S, F32)
            DM = ar.take(1024, BF16).rearrange("p (v n) -> p v n", v=2)
            PTs = [ar.take(512, BF16) for _ in range(3)]
            PTR = Rot([(PTs[i], "PT%d" % i) for i in range(3)])
            SQ = ar.take(512, BF16); RS = ar.take(512, F32)
            RL = ar.take(512, F32); T1 = ar.take(512, F32); ZSb = ar.take(512, BF16)
            MH = [ar.take(S, BF16) for _ in range(2)]
            DMA("sp", DM, dr["dm"], [], ["DM"])
            for hh in range(4):
                for gi, dil in enumerate((1, 4, 16)):
                    L = S // dil
                    nqt = L // 128

                    def sub(buf, c, qt, dil=dil):
                        return buf.rearrange("p (a d) -> p a d", d=dil)[:, 128 * qt:128 * qt + 128, c]

                    w = next_w()
                    for tb in range(4):
                        ps, pt = proj_fm(w, tb)
                        norm_fm(ps, pt, 512, prm("dqg", 0), QN[:, tb * 512:(tb + 1) * 512], "QN", SQ, RS)
                    w = next_w()
                    for tb in range(4):
                        ps, pt = proj_fm(w, tb)
                        norm_fm(ps, pt, 512, prm("dkg", 0), KN[:, tb * 512:(tb + 1) * 512], "KN", SQ, RS)
                    w = next_w()
                    QL = [(c, qt) for c in range(dil) for qt in range(nqt)]
                    for t4 in range(4):
                        taps = []
                        for j in range(4):
                            c, qt = QL[4 * t4 + j]
                            taps.append(lambda kc, c=c, qt=qt: sub(XNT[:, kc, :], c, qt))
                        ps, pt = proj_tm(w, taps)
                        ACT(VD[:, 4 * t4:4 * t4 + 4, :], ps[:, :].rearrange("p (a b) -> p a b", a=4), AF.Copy, [pt], ["VD"])
                    for q4 in range(4):
                        oa, oat = OA.next(); la, lat = LA.next()
                        for half in range(2):
                            st, sk = ST.next()
                            info = []
                            for u in range(2):
                                ti = 4 * q4 + 2 * half + u
                                c, qt = QL[ti]
                                tp = ti - 1 if qt > 0 else ti
                                cp, qp = QL[tp]
                                qap = sub(QN, c, qt)
                                mm(st[:, u * 256:u * 256 + 128], sub(KN, cp, qp), qap, True, True, ["KN", "QN"], [sk])
                                mm(st[:, u * 256 + 128:u * 256 + 256], sub(KN, c, qt), qap, True, True, ["KN", "QN"], [sk])
                                info.append((ti, tp, qt))
                            pa, pk = PTR.next()
                            ACT(pa, st[:, :], AF.Exp, [sk], [pk], scale=float(SCALE))
                            for u in range(2):
                                ti, tp, qt = info[u]
                                v = 0 if qt > 0 else 1
                                TT(pa[:, u * 256:u * 256 + 256], pa[:, u * 256:u * 256 + 256], DM[:, v, 0:256] if v == 0 else DM[:, 1, 0:256],
                                   MUL, [pk, "DM"], [pk])
                            for u in range(2):
                                ti, tp, qt = info[u]
                                col = (2 * half + u) * 128
                                mm(oa[:, col:col + 128], VD[:, tp, :], pa[:, u * 256:u * 256 + 128], True, False, ["VD", pk], [oat])
                                mm(oa[:, col:col + 128], VD[:, ti, :], pa[:, u * 256 + 128:u * 256 + 256], False, True, ["VD", pk], [oat])
                                mm(la[:, col:col + 128], ONESB[:, :], pa[:, u * 256:u * 256 + 128], True, False, ["ONESB", pk], [lat])
                                mm(la[:, col:col + 128], ONESB[:, :], pa[:, u * 256 + 128:u * 256 + 256], False, True, ["ONESB", pk], [lat])
                        if nqt >= 4:
                            c = (4 * q4) // nqt
                            qt0 = (4 * q4) % nqt

                            def dst(buf, c=c, qt0=qt0, dil=dil):
                                return buf.rearrange("p (a d) -> p a d", d=dil)[:, 128 * qt0:128 * qt0 + 512, c]

                            srcv = lambda p_: p_[:, :]
                        else:
                            def dst(buf, q4=q4):
                                return buf.rearrange("p (a d) -> p d a", d=16)[:, 4 * q4:4 * q4 + 4, :]

                            srcv = lambda p_: p_[:, :].rearrange("p (u i) -> p u i", u=4)
                        if gi == 0:
                            ACT(dst(OACC), srcv(oa), AF.Copy, [oat], ["OACC"])
                            ACT(dst(LACC), srcv(la), AF.Copy, [lat], ["LACC"])
                        else:
                            TT(dst(OACC), srcv(oa), dst(OACC), ADD, [oat, "OACC"], ["OACC"])
                            TT(dst(LACC), srcv(la), dst(LACC), ADD, [lat, "LACC"], ["LACC"])
                w = next_w()
                mh = MH[hh % 2]; mhk = "MH%d" % (hh % 2)
                for tb in range(4):
                    blk = slice(tb * 512, (tb + 1) * 512)
                    pz, pzt = proj_fm(w, tb)
                    ACT(ZSb, pz[:, :], AF.Silu, [pzt], ["ZSb"])
                    ACT(RL, LACC[:, blk], AF.Ln, ["LACC"], ["RL"])
                    ACT(RL, RL, AF.Exp, ["RL"], ["RL"], scale=-1.0)
                    TT(T1, OACC[:, blk], RL, MUL, ["OACC", "RL"], ["T1"])
                    TT(mh[:, blk], T1, ZSb, MUL, ["T1", "ZSb"], [mhk])
                DMA("sp", mixd[8 + hh], mh, [mhk], ["mixd"])

        def phaseC(seq, xin_d, xout_d, mixd, wo, nmc, is_out):
            ar.reset(0)
            WO = ar.take(4 * nmc * 512, BF16).rearrange("p (b c n) -> p b c n", b=4, c=nmc)
            XR = [ar.take(D, F32) for _ in range(2)]
            Y = [ar.take(D, F32) for _ in range(2)]
            MT = [ar.take(nmc * 128, BF16).rearrange("p (c t) -> p c t", c=nmc) for _ in range(2)]
            for nb in range(4):
                DMA("pool", WO[:, nb], wo[nb], [], ["WO%d" % nb])

            def loads(tt):
                b = tt % 2
                r0 = seq * S + tt * 128
                DMA("sp", MT[b], mixd[0:nmc, :, tt * 128:(tt + 1) * 128].rearrange("c p t -> p c t"), ["mixd"], ["MT%d" % b])
                DMA("sp", XR[b], xin_d[r0:r0 + 128, :], [], ["XR%d" % b])

            loads(0)
            for tt in range(16):
                b = tt % 2
                r0 = seq * S + tt * 128
                if tt + 1 < 16:
                    loads(tt + 1)
                for nb in range(4):
                    ps, pt = PJ.next()
                    for mc in range(nmc):
                        mm(ps[:, :], MT[b][:, mc, :], WO[:, nb, mc, :], mc == 0, mc == nmc - 1, ["MT%d" % b, "WO%d" % nb], [pt])
                    TT(Y[b][:, nb * 512:(nb + 1) * 512], ps[:, :], XR[b][:, nb * 512:(nb + 1) * 512], ADD,
                       [pt, "XR%d" % b], ["Y%d" % b])
                DMA("sp", xout_d[r0:r0 + 128, :], Y[b], ["Y%d" % b], ["xout"], is_out=is_out)

        XNT = ar.take(16 * S, BF16).rearrange("p (k n) -> p k n", k=16)
        nl = len(layers)
        for seq in range(nseq):
            for li, ly in enumerate(layers):
                xin = x_d if li == 0 else x1_d
                xout = out_d if li == nl - 1 else x1_d
                mixd = mix_d[seq * 2 + li]
                on = lambda nm: phases is None or nm in phases
                P.fence()
                if on("A"):
                    phaseA(xin, seq, "g%d" % ly)
                P.fence()
                if ly == 0:
                    if on("ret"):
                        retention(seq, mixd)
                    else:
                        wstate["next"] += 32; wstate["loaded"] = max(wstate["loaded"], wstate["next"])
                    P.fence()
                    if on("nsa"):
                        nsa(seq, mixd)
                else:
                    if on("conv"):
                        conv_branch(seq, mixd)
                    else:
                        wstate["next"] += 24; wstate["loaded"] = max(wstate["loaded"], wstate["next"])
                    P.fence()
                    if on("dil"):
                        dilated(seq, mixd)
                P.fence()
                if on("C"):
                    phaseC(seq, xin, xout, mixd, dr["wo%d" % ly], 16 if ly == 0 else 12, li == nl - 1)
        P.emit()
    return nc


_CACHE = {}


def _prep(inputs, nseq, ncores):
    consts = host_consts()
    params = host_params(inputs)
    x = np.asarray(inputs["x"], np.float32)
    maps = []
    for c in range(ncores):
        m = {"x": np.ascontiguousarray(x[c * nseq:(c + 1) * nseq].reshape(nseq * S, D))}
        for k, v in consts.items():
            m["c_" + k] = v
        for k, v in params.items():
            m["p_" + k] = v
        maps.append(m)
    return maps


def kernel(**inputs):
    ncores, nseq = 8, 2
    if "nc" not in _CACHE:
        _CACHE["nc"] = build(nseq=nseq, layers=(0, 1))
    nc = _CACHE["nc"]
    maps = _prep(inputs, nseq, ncores)
    res = run_bass_kernel_spmd(nc, maps, core_ids=list(range(ncores)))
    outs = [np.asarray(r["out"], np.float32).reshape(nseq, S, D) for r in res.results]
    return np.concatenate(outs, axis=0)
```
